# Optimizing a Trainium2 kernel written in Bass

```python
import jax, jax.numpy as jnp
from jax import lax
import numpy as np

D_MODEL = 1024
BATCH = 4
SEQ = 4096
DEPTH = 4

GRID_W = 64
CTX_LEN = 256
EPS = 1e-6
N_MOD = 6
N_BRANCH = 3
GLA_HEADS = 4
GLA_DK = 64
GLA_DV = 128
GLA_RANK = 16
GLA_TAU = 16.0
GLA_CHUNK = 64
GLA_SCALE = GLA_DK ** -0.5
FNET_GROUPS = 4
FNET_GROUP_W = 128
SWA_HEADS = 8
SWA_KV_HEADS = 2
SWA_HEAD_DIM = 64
SWA_GROUP = SWA_HEADS // SWA_KV_HEADS
WINDOW = 128
ROPE_BASE = 10000.0
AXIS_DIM = SWA_HEAD_DIM // 2
MOE_GROUPS = 4
MOE_EXPERTS_PER_GROUP = 4
MOE_EXPERTS = MOE_GROUPS * MOE_EXPERTS_PER_GROUP
MOE_TOPK = 2
D_EXPERT = 512

GLA_QK_W = GLA_HEADS * GLA_DK
GLA_V_W = GLA_HEADS * GLA_DV
FNET_W = FNET_GROUPS * FNET_GROUP_W
SWA_Q_W = SWA_HEADS * SWA_HEAD_DIM
SWA_KV_W = SWA_KV_HEADS * SWA_HEAD_DIM
IN_SIZES = [GLA_QK_W, GLA_V_W, SWA_KV_W, SWA_KV_W, GLA_QK_W, GLA_V_W, FNET_W, SWA_Q_W, N_BRANCH * D_MODEL]
IN_COLS = sum(IN_SIZES)

kernel_name = "hybrid_gla_fourier_swa_hmoe_dit"


def rms_norm(x, g):
    xf = x.astype(jnp.float32)
    y = xf * lax.rsqrt(jnp.mean(xf * xf, axis=-1, keepdims=True) + EPS)
    return (y * g.astype(jnp.float32)).astype(x.dtype)


def modulate(x, shift, scale):
    return x * (1 + scale[..., None, :]) + shift[..., None, :]


def split_by(t, sizes):
    return jnp.split(t, [int(i) for i in np.cumsum(sizes)[:-1]], axis=-1)


def heads(t, n):
    return t.reshape(t.shape[0], t.shape[1], n, -1)


def flip_seq(t):
    return jnp.flip(t, axis=1)


def gla_log_decay(h, w_down, w_up, b):
    z = (h @ w_down) @ w_up + b
    la = jax.nn.log_sigmoid(z.astype(jnp.float32)) / GLA_TAU
    return la.reshape(h.shape[0], h.shape[1], GLA_HEADS, GLA_DK)


def gla_chunked(q, k, v, log_a, s0):
    b_, l_, h_, _ = q.shape
    n = l_ // GLA_CHUNK

    def to_chunks(t):
        t = t.astype(jnp.float32)
        return jnp.moveaxis(t.reshape(b_, n, GLA_CHUNK, *t.shape[2:]), 1, 0)

    lower = jnp.tril(jnp.ones((GLA_CHUNK, GLA_CHUNK), dtype=bool))[None, :, :, None, None]

    def step(state, inp):
        qc, kc, vc, ac = inp
        cum = jnp.cumsum(ac, axis=1)
        inter = jnp.einsum('bchk,bhkv->bchv', qc * jnp.exp(cum), state)
        diff = cum[:, :, None] - cum[:, None, :]
        decay = jnp.exp(jnp.where(lower, diff, -jnp.inf))
        scores = jnp.einsum('bthk,bshk,btshk->btsh', qc, kc, decay)
        intra = jnp.einsum('btsh,bshv->bthv', scores, vc)
        last = cum[:, -1]
        new_state = jnp.exp(last)[..., None] * state + jnp.einsum(
            'bshk,bshv->bhkv', kc * jnp.exp(last[:, None] - cum), vc)
        return new_state, inter + intra

    state, out = lax.scan(step, s0, (to_chunks(q), to_chunks(k), to_chunks(v), to_chunks(log_a)))
    out = jnp.moveaxis(out, 0, 1).reshape(b_, l_, h_, v.shape[-1])
    return out, state


def gla_final_state(k, v, log_a):
    cum = jnp.cumsum(log_a.astype(jnp.float32), axis=1)
    return jnp.einsum('bshk,bshv->bhkv', k.astype(jnp.float32) * jnp.exp(cum[:, -1:] - cum),
                      v.astype(jnp.float32))


def gla_bidirectional(q, k, v, la_f, la_b, s0_f, s0_b):
    o_f, s_f = gla_chunked(q, k, v, la_f, s0_f)
    o_b, s_b = gla_chunked(flip_seq(q), flip_seq(k), flip_seq(v), flip_seq(la_b), s0_b)
    return o_f + flip_seq(o_b), s_f, s_b


def gla_output(o, r, g_gla):
    o = rms_norm(o, g_gla).reshape(o.shape[0], o.shape[1], GLA_V_W)
    return (o * jax.nn.silu(r.astype(jnp.float32))).astype(r.dtype)


def fourier_mix(u):
    b_, l_, _ = u.shape
    ug = u.astype(jnp.float32).reshape(b_, l_, FNET_GROUPS, FNET_GROUP_W)
    f = jnp.fft.fftn(ug, axes=(1, 3), norm='ortho').real
    return f.reshape(b_, l_, FNET_W).astype(u.dtype)


def axial_angles(n_tokens):
    rows = n_tokens // GRID_W
    row = jnp.repeat(jnp.arange(rows, dtype=jnp.float32), GRID_W)
    col = jnp.tile(jnp.arange(GRID_W, dtype=jnp.float32), rows)
    inv_freq = ROPE_BASE ** (-jnp.arange(0, AXIS_DIM, 2, dtype=jnp.float32) / AXIS_DIM)
    return row[:, None] * inv_freq, col[:, None] * inv_freq


def rope_1d(x, ang):
    x1, x2 = jnp.split(x.astype(jnp.float32), 2, axis=-1)
    cos = jnp.cos(ang)[None, :, None, :]
    sin = jnp.sin(ang)[None, :, None, :]
    return jnp.concatenate([x1 * cos - x2 * sin, x1 * sin + x2 * cos], axis=-1)


def rope_2d(x, ang_row, ang_col):
    xr, xc = jnp.split(x, 2, axis=-1)
    return jnp.concatenate([rope_1d(xr, ang_row), rope_1d(xc, ang_col)], axis=-1).astype(x.dtype)


def windowed_gqa_with_context(q, k, v, kc, vc, sink):
    b_, l_, _, d = q.shape
    nb = l_ // WINDOW
    scale = d ** -0.5
    qb = q.reshape(b_, nb, WINDOW, SWA_KV_HEADS, SWA_GROUP, d)
    pad = ((0, 0), (WINDOW, WINDOW), (0, 0), (0, 0))
    kb = jnp.pad(k, pad).reshape(b_, nb + 2, WINDOW, SWA_KV_HEADS, d)
    vb = jnp.pad(v, pad).reshape(b_, nb + 2, WINDOW, SWA_KV_HEADS, d)
    kw = jnp.concatenate([kb[:, :-2], kb[:, 1:-1], kb[:, 2:]], axis=2)
    vw = jnp.concatenate([vb[:, :-2], vb[:, 1:-1], vb[:, 2:]], axis=2)
    a = jnp.arange(WINDOW)[:, None]
    j = jnp.arange(3 * WINDOW)[None, :]
    band = (j - a >= 0) & (j - a <= 2 * WINDOW)
    key_pos = (jnp.arange(nb)[:, None, None] - 1) * WINDOW + j[None]
    valid = band[None] & (key_pos >= 0) & (key_pos < l_)
    s_win = jnp.einsum('bnqhgd,bnkhd->bnhgqk', qb, kw).astype(jnp.float32) * scale
    s_win = jnp.where(valid[None, :, None, None], s_win, -jnp.inf)
    s_ctx = jnp.einsum('bnqhgd,bkhd->bnhgqk', qb, kc).astype(jnp.float32) * scale
    s_sink = jnp.broadcast_to(
        sink.astype(jnp.float32).reshape(SWA_KV_HEADS, SWA_GROUP)[None, None, :, :, None, None],
        s_win.shape[:-1] + (1,))
    probs = jax.nn.softmax(jnp.concatenate([s_win, s_ctx, s_sink], axis=-1), axis=-1)
    n_win = 3 * WINDOW
    p_win = probs[..., :n_win].astype(v.dtype)
    p_ctx = probs[..., n_win:n_win + kc.shape[1]].astype(v.dtype)
    out = (jnp.einsum('bnhgqk,bnkhd->bnqhgd', p_win, vw)
           + jnp.einsum('bnhgqk,bkhd->bnqhgd', p_ctx, vc))
    return out.reshape(b_, l_, SWA_Q_W)


def context_gqa(qc, kc, vc, sink):
    b_, n_, _, d = qc.shape
    q = qc.reshape(b_, n_, SWA_KV_HEADS, SWA_GROUP, d)
    s = jnp.einsum('bqhgd,bkhd->bhgqk', q, kc).astype(jnp.float32) * d ** -0.5
    s_sink = jnp.broadcast_to(
        sink.astype(jnp.float32).reshape(SWA_KV_HEADS, SWA_GROUP)[None, :, :, None, None],
        s.shape[:-1] + (1,))
    p = jax.nn.softmax(jnp.concatenate([s, s_sink], axis=-1), axis=-1)[..., :-1].astype(vc.dtype)
    return jnp.einsum('bhgqk,bkhd->bqhgd', p, vc).reshape(b_, n_, SWA_Q_W)


def merge_branches(y_a, y_b, y_c, gate_logits, w_pa, w_pb, w_pc, w_out):
    g_a, g_b, g_c = jnp.split(jax.nn.sigmoid(gate_logits), N_BRANCH, axis=-1)
    return (g_a * (y_a @ w_pa) + g_b * (y_b @ w_pb) + g_c * (y_c @ w_pc)) @ w_out


def hybrid_mixer(h, hc, w_in, w_decay_down, w_decay_up, b_decay, g_gla, sink, w_pa, w_pb, w_pc, w_out, last):
    b_, l_, _ = h.shape
    k_a, v_a, k_c, v_c, q_a, r_a, u_b, q_c, gate_logits = split_by(h @ w_in, IN_SIZES)
    n_parts = 4 if last else len(IN_SIZES)
    ctx_sizes = IN_SIZES[:n_parts]
    cparts = split_by(hc @ w_in[:, :sum(ctx_sizes)], ctx_sizes)
    ck_a, cv_a = heads(cparts[0], GLA_HEADS), heads(cparts[1], GLA_HEADS)
    ck_c, cv_c = heads(cparts[2], SWA_KV_HEADS), heads(cparts[3], SWA_KV_HEADS)

    la_f = gla_log_decay(h, w_decay_down[0], w_decay_up[0], b_decay[0])
    la_b = gla_log_decay(h, w_decay_down[1], w_decay_up[1], b_decay[1])
    cla_f = gla_log_decay(hc, w_decay_down[0], w_decay_up[0], b_decay[0])
    cla_b = gla_log_decay(hc, w_decay_down[1], w_decay_up[1], b_decay[1])
    if last:
        s_f = gla_final_state(ck_a, cv_a, cla_f)
        s_b = gla_final_state(flip_seq(ck_a), flip_seq(cv_a), flip_seq(cla_b))
    else:
        s0 = jnp.zeros((b_, GLA_HEADS, GLA_DK, GLA_DV), jnp.float32)
        co_a, s_f, s_b = gla_bidirectional(heads(cparts[4], GLA_HEADS) * GLA_SCALE, ck_a, cv_a,
                                           cla_f, cla_b, s0, s0)
    o_a, _, _ = gla_bidirectional(heads(q_a, GLA_HEADS) * GLA_SCALE, heads(k_a, GLA_HEADS),
                                  heads(v_a, GLA_HEADS), la_f, la_b, s_f, s_b)
    y_a = gla_output(o_a, r_a, g_gla)

    y_b = fourier_mix(u_b)

    ang_row, ang_col = axial_angles(l_)
    q_rot = rope_2d(heads(q_c, SWA_HEADS), ang_row, ang_col)
    k_rot = rope_2d(heads(k_c, SWA_KV_HEADS), ang_row, ang_col)
    y_c = windowed_gqa_with_context(q_rot, k_rot, heads(v_c, SWA_KV_HEADS), ck_c, cv_c, sink)

    y = merge_branches(y_a, y_b, y_c, gate_logits, w_pa, w_pb, w_pc, w_out)
    if last:
        return y, None
    yc_a = gla_output(co_a, cparts[5], g_gla)
    yc_b = fourier_mix(cparts[6])
    yc_c = context_gqa(heads(cparts[7], SWA_HEADS), ck_c, cv_c, sink)
    yc = merge_branches(yc_a, yc_b, yc_c, cparts[8], w_pa, w_pb, w_pc, w_out)
    return y, yc


def hierarchical_moe(h, w_router_group, w_router_expert, w1, w3, w2):
    g_logits = (h @ w_router_group).astype(jnp.float32)
    g_sel = jnp.argmax(g_logits, axis=-1)
    g_gate = jnp.take_along_axis(jax.nn.softmax(g_logits, axis=-1), g_sel[:, None], axis=-1)
    e_logits = (h @ w_router_expert).astype(jnp.float32).reshape(-1, MOE_GROUPS, MOE_EXPERTS_PER_GROUP)
    e_in_group = jnp.take_along_axis(e_logits, g_sel[:, None, None], axis=1)[:, 0]
    top_vals, top_idx = lax.top_k(e_in_group, MOE_TOPK)
    top_w = jax.nn.softmax(top_vals, axis=-1) * g_gate
    expert_id = g_sel[:, None] * MOE_EXPERTS_PER_GROUP + top_idx
    gates = jnp.sum(jax.nn.one_hot(expert_id, MOE_EXPERTS, dtype=jnp.float32) * top_w[..., None], axis=1)
    gates = gates.astype(h.dtype)
    out = jnp.zeros(h.shape, h.dtype)
    for e in range(MOE_EXPERTS):
        hid = jax.nn.silu(h @ w1[e]) * (h @ w3[e])
        out = out + (hid @ w2[e]) * gates[:, e:e + 1]
    return out


def trunk_layer(x, xc, c_act, cc_act, w_ada, b_ada, g_mix, g_ffn, w_in, w_decay_down, w_decay_up, b_decay,
                g_gla, sink, w_pa, w_pb, w_pc, w_out, w_router_group, w_router_expert, w1, w3, w2, last):
    shift1, scale1, gate1, shift2, scale2, gate2 = jnp.split(c_act @ w_ada + b_ada, N_MOD, axis=-1)
    n_ctx_mod = 2 if last else N_MOD
    mods_c = jnp.split(cc_act @ w_ada[:, :n_ctx_mod * D_MODEL] + b_ada[:n_ctx_mod * D_MODEL], n_ctx_mod, axis=-1)
    h = modulate(rms_norm(x, g_mix), shift1, scale1)
    hc = modulate(rms_norm(xc, g_mix), mods_c[0], mods_c[1])
    y, yc = hybrid_mixer(h, hc, w_in, w_decay_down, w_decay_up, b_decay, g_gla, sink,
                         w_pa, w_pb, w_pc, w_out, last)
    x = x + gate1[:, None, :] * y
    h2 = modulate(rms_norm(x, g_ffn), shift2, scale2)
    x = x + gate2[:, None, :] * hierarchical_moe(h2.reshape(-1, D_MODEL), w_router_group, w_router_expert,
                                                 w1, w3, w2).reshape(x.shape)
    if last:
        return x, None
    xc = xc + mods_c[2] * yc
    hc2 = modulate(rms_norm(xc, g_ffn), mods_c[3], mods_c[4])
    xc = xc + mods_c[5] * hierarchical_moe(hc2.reshape(-1, D_MODEL), w_router_group, w_router_expert,
                                           w1, w3, w2).reshape(xc.shape)
    return x, xc


def setup_inputs(seed: int = 0) -> dict:
    key = jax.random.key(seed)
    ks = jax.random.split(key, 24)
    D = D_MODEL

    def nrm(k, shape, s):
        return jax.random.normal(k, shape, jnp.float32) * s

    return {
        'x': nrm(ks[0], (BATCH, SEQ, D), 1.0),
        'c': nrm(ks[1], (BATCH, D), 1.0),
        'ctx': nrm(ks[2], (BATCH, CTX_LEN, D), 1.0),
        'c_ctx': nrm(ks[3], (D,), 1.0),
        'w_ada': nrm(ks[4], (DEPTH, D, N_MOD * D), 0.5 * D ** -0.5),
        'b_ada': nrm(ks[5], (DEPTH, N_MOD * D), 0.02),
        'g_mix': 1.0 + nrm(ks[6], (DEPTH, D), 0.05),
        'g_ffn': 1.0 + nrm(ks[7], (DEPTH, D), 0.05),
        'w_in': nrm(ks[8], (DEPTH, D, IN_COLS), D ** -0.5),
        'w_decay_down': nrm(ks[9], (DEPTH, 2, D, GLA_RANK), D ** -0.5),
        'w_decay_up': nrm(ks[10], (DEPTH, 2, GLA_RANK, GLA_QK_W), GLA_RANK ** -0.5),
        'b_decay': nrm(ks[11], (DEPTH, 2, GLA_QK_W), 0.5),
        'g_gla': 1.0 + nrm(ks[12], (DEPTH, GLA_DV), 0.05),
        'sink': nrm(ks[13], (DEPTH, SWA_HEADS), 0.5),
        'w_pa': nrm(ks[14], (DEPTH, GLA_V_W, D), GLA_V_W ** -0.5),
        'w_pb': nrm(ks[15], (DEPTH, FNET_W, D), FNET_W ** -0.5),
        'w_pc': nrm(ks[16], (DEPTH, SWA_Q_W, D), SWA_Q_W ** -0.5),
        'w_out': nrm(ks[17], (DEPTH, D, D), D ** -0.5),
        'w_router_group': nrm(ks[18], (DEPTH, D, MOE_GROUPS), D ** -0.5),
        'w_router_expert': nrm(ks[19], (DEPTH, D, MOE_EXPERTS), D ** -0.5),
        'w1': nrm(ks[20], (DEPTH, MOE_EXPERTS, D, D_EXPERT), D ** -0.5),
        'w3': nrm(ks[21], (DEPTH, MOE_EXPERTS, D, D_EXPERT), D ** -0.5),
        'w2': nrm(ks[22], (DEPTH, MOE_EXPERTS, D_EXPERT, D), D_EXPERT ** -0.5),
        'g_final': 1.0 + nrm(ks[23], (D,), 0.05),
    }


def reference(x, c, ctx, c_ctx, w_ada, b_ada, g_mix, g_ffn, w_in, w_decay_down, w_decay_up, b_decay, g_gla,
              sink, w_pa, w_pb, w_pc, w_out, w_router_group, w_router_expert, w1, w3, w2, g_final):
    c_act = jax.nn.silu(c)
    cc_act = jax.nn.silu(c_ctx)
    xc = ctx
    for l in range(DEPTH):
        x, xc = trunk_layer(x, xc, c_act, cc_act, w_ada[l], b_ada[l], g_mix[l], g_ffn[l], w_in[l],
                            w_decay_down[l], w_decay_up[l], b_decay[l], g_gla[l], sink[l], w_pa[l], w_pb[l],
                            w_pc[l], w_out[l], w_router_group[l], w_router_expert[l], w1[l], w3[l], w2[l],
                            l == DEPTH - 1)
    return rms_norm(x, g_final)
```

```python
import math
from contextlib import ExitStack

import numpy as np
import concourse.bass as bass
import concourse.mybir as mybir
from concourse.bass_utils import run_bass_kernel_spmd

F32 = mybir.dt.float32
BF16 = mybir.dt.bfloat16
AF = mybir.ActivationFunctionType
ALU = mybir.AluOpType
AX = mybir.AxisListType


class Sched:
    EPOCH = 30000
    NDSEM = 24
    SYNC_LAT = 0.9
    DMA_LAT = 2.2

    def __init__(self, nc, same_engine_sync=True, reorder=True):
        self.nc = nc
        self.engs = {"pe": nc.tensor, "dve": nc.vector, "act": nc.scalar, "pool": nc.gpsimd, "sp": nc.sync}
        self.same = same_engine_sync
        self.reorder = reorder
        self.ops = []
        self.segments = []
        self.last_w = {}
        self.readers = {}
        self.n_ins = 0

    @staticmethod
    def _k(r):
        if isinstance(r, (str, int)):
            return r
        if isinstance(r, tuple):
            return tuple(Sched._k(x) for x in r)
        return r.name

    def _record(self, e, fn, kind, reads, writes, dur, lat):
        reads = [self._k(r) for r in reads]
        writes = [self._k(r) for r in writes]
        deps = set()
        for r in reads:
            w = self.last_w.get(r)
            if w is not None:
                deps.add(w)
        for w_ in writes:
            w = self.last_w.get(w_)
            if w is not None:
                deps.add(w)
            rd = self.readers.get(w_)
            if rd:
                deps.update(rd)
        idx = len(self.ops)
        self.ops.append(dict(e=e, fn=fn, kind=kind, deps=deps, dur=dur, lat=lat))
        for r in reads:
            self.readers.setdefault(r, []).append(idx)
        for w_ in writes:
            self.last_w[w_] = idx
            self.readers[w_] = []
        self.n_ins += 1
        return idx

    def op(self, e, fn, reads=(), writes=(), dur=0.3):
        return self._record(e, fn, "c", reads, writes, dur, dur)

    def dma(self, q, out, in_, reads=(), writes=(), nbytes=0, **kw):
        if not nbytes:
            nbytes = 1
            for d in out.shape:
                nbytes *= int(d)
            nbytes *= 2 if out.dtype == BF16 else 4
        lat = self.DMA_LAT + nbytes / 150e3
        return self._record(q, lambda eng: eng.dma_start(out=out, in_=in_, **kw), "d", reads, writes, 0.06, lat)

    def collective(self, kind, op, out, in_, groups, reads=(), writes=()):
        return self._record("pool", lambda eng: eng.collective_compute(kind, op, replica_groups=groups, ins=[in_], outs=[out]),
                            "cc", reads, writes, 0.1, 80.0)

    def wait_all(self, e, resources):
        return self._record(e, None, "w", list(resources), [], 0.01, 0.01)

    def barrier(self):
        if self.ops:
            self.segments.append(self.ops)
        self.ops = []
        self.last_w.clear()
        self.readers.clear()

    def _schedule(self, ops):
        import heapq
        n = len(ops)
        order = {e: [] for e in self.engs}
        if not self.reorder:
            for i, o in enumerate(ops):
                order[o["e"]].append(i)
            return order
        children = [[] for _ in range(n)]
        indeg = [0] * n
        for i, o in enumerate(ops):
            indeg[i] = len(o["deps"])
            for d in o["deps"]:
                children[d].append(i)
        finish = [0.0] * n
        ready_t = [0.0] * n
        ht = {e: [] for e in self.engs}
        ha = {e: [] for e in self.engs}
        tfree = {e: 0.0 for e in self.engs}
        for i, o in enumerate(ops):
            if indeg[i] == 0:
                heapq.heappush(ht[o["e"]], (0.0, i))
        done = 0
        while done < n:
            best = None
            for e in self.engs:
                h, a = ht[e], ha[e]
                tf = tfree[e]
                while h and h[0][0] <= tf:
                    heapq.heappush(a, heapq.heappop(h)[1])
                if a:
                    cand = (tf, a[0], e, True)
                elif h:
                    cand = (h[0][0], h[0][1], e, False)
                else:
                    continue
                if best is None or cand[:2] < best[:2]:
                    best = cand
            start, i, e, from_a = best
            if from_a:
                heapq.heappop(ha[e])
            else:
                heapq.heappop(ht[e])
            o = ops[i]
            tfree[e] = start + o["dur"]
            finish[i] = start + o["lat"]
            order[e].append(i)
            done += 1
            for c in children[i]:
                oc = ops[c]
                t = finish[i] + (self.SYNC_LAT if oc["e"] != e else 0.05)
                if t > ready_t[c]:
                    ready_t[c] = t
                indeg[c] -= 1
                if indeg[c] == 0:
                    heapq.heappush(ht[oc["e"]], (ready_t[c], c))
        return order

    def emit(self):
        nc = self.nc
        self.barrier()
        count = {e: 0 for e in self.engs}
        known = {e: {} for e in self.engs}
        dma_cnt = {}
        dma_i = {}
        semkeys = set()
        prog = {e: [] for e in self.engs}
        for ops in self.segments:
            order = self._schedule(ops)
            ev = [None] * len(ops)
            extra = [None] * len(ops)
            for e in self.engs:
                for i in order[e]:
                    o = ops[i]
                    if o["kind"] == "c":
                        idx = count[e]
                        count[e] += 1
                        key = ("c", e, idx // self.EPOCH)
                        ev[i] = (key, idx % self.EPOCH + 1)
                        semkeys.add(key)
                    elif o["kind"] == "cc":
                        key = ("d", "cc", 0)
                        if dma_cnt.get(key, 0) > 0:
                            extra[i] = (key, 16 * dma_cnt[key])
                        dma_cnt[key] = dma_cnt.get(key, 0) + 1
                        ev[i] = (key, 16 * dma_cnt[key])
                        semkeys.add(key)
                    elif o["kind"] == "d":
                        nsem = self.NDSEM if e == "sp" else 8
                        j = dma_i.get(e, 0) % nsem
                        dma_i[e] = dma_i.get(e, 0) + 1
                        key = ("d", e, j)
                        if dma_cnt.get(key, 0) > 0:
                            extra[i] = (key, 16 * dma_cnt[key])
                        dma_cnt[key] = dma_cnt.get(key, 0) + 1
                        ev[i] = (key, 16 * dma_cnt[key])
                        semkeys.add(key)
            for e in self.engs:
                kn = known[e]
                for i in order[e]:
                    o = ops[i]
                    best = {}
                    for d in o["deps"]:
                        od = ops[d]
                        if ev[d] is None:
                            continue
                        if od["e"] == e and od["kind"] == "c" and o["kind"] == "c" and (e == "pe" or not self.same):
                            continue
                        k, v = ev[d]
                        if best.get(k, 0) < v:
                            best[k] = v
                    if extra[i] is not None:
                        k, v = extra[i]
                        if best.get(k, 0) < v:
                            best[k] = v
                    waits = []
                    for k, v in best.items():
                        if kn.get(k, 0) >= v:
                            continue
                        kn[k] = v
                        waits.append((k, v))
                    if o["kind"] == "w":
                        if waits:
                            prog[e].append((waits, None, None, 0))
                    else:
                        prog[e].append((waits, o["fn"], ev[i][0], 1 if o["kind"] == "c" else 16))
            evs = []
            for e in self.engs:
                if count[e] > 0:
                    evs.append((("c", e, (count[e] - 1) // self.EPOCH), (count[e] - 1) % self.EPOCH + 1))
            for key, c in dma_cnt.items():
                evs.append((key, 16 * c))
            for e in self.engs:
                kn = known[e]
                waits = []
                for k, v in evs:
                    if k[0] == "c" and k[1] == e:
                        continue
                    if kn.get(k, 0) >= v:
                        continue
                    kn[k] = v
                    waits.append((k, v))
                if waits:
                    prog[e].append((waits, None, None, 0))
        self.prog = prog
        with ExitStack() as st:
            sems = {}
            for k in sorted(semkeys, key=str):
                sems[k] = st.enter_context(nc.semaphore("s_" + "_".join(str(x) for x in k)))
            block = st.enter_context(nc.Block())

            def mk(ename):
                def body(eng):
                    for waits, fn, key, inc in prog[ename]:
                        for k, v in waits:
                            eng.wait_ge(sems[k], v)
                        if fn is not None:
                            fn(eng).then_inc(sems[key], inc)
                return body

            block.sync(mk("sp"))
            block.tensor(mk("pe"))
            block.vector(mk("dve"))
            block.scalar(mk("act"))
            block.gpsimd(mk("pool"))


U8 = mybir.dt.uint8
_DTSZ = {F32: 4, BF16: 2}


class Tile:
    def __init__(self, name, ap):
        self.name = name
        self.ap = ap

    def __getitem__(self, k):
        return self.ap[k]


class Arena:
    def __init__(self, nc, st, nbytes=206 * 1024):
        self.t = st.enter_context(nc.sbuf_tensor("arena", [128, nbytes], U8))
        self.nbytes = nbytes
        self.off = 0
        self.marks = []
        self.n = 0

    def push(self):
        self.marks.append(self.off)

    def pop(self):
        self.off = self.marks.pop()

    def tile(self, name, shape, dt):
        free = int(np.prod(shape[1:]))
        nb = (free * _DTSZ[dt] + 31) // 32 * 32
        assert self.off + nb <= self.nbytes, f"SBUF arena overflow at {name}: {self.off}+{nb}"
        v = self.t[0:shape[0], self.off:self.off + free * _DTSZ[dt]].bitcast(dt)
        self.off += nb
        if len(shape) == 3:
            v = v.rearrange("p (a b) -> p a b", a=shape[1])
        elif len(shape) == 4:
            v = v.rearrange("p (a b c) -> p a b c", a=shape[1], b=shape[2])
        self.n += 1
        return Tile(f"{name}#{self.n}", v)


D = 1024
CTX = 256
EPS = 1e-6
IN_COLS = 5888
C_KA, C_VA, C_KC, C_VC, C_QA, C_RA, C_UB, C_QC, C_G = 0, 256, 768, 896, 1024, 1280, 1792, 2304, 2816


def host_consts(L):
    import ml_dtypes
    bf = ml_dtypes.bfloat16
    T = L + CTX
    c = {}
    c["identb"] = np.eye(128, dtype=np.float32).astype(bf)
    c["identf"] = np.eye(128, dtype=np.float32)
    s = np.arange(128)[:, None]
    t = np.arange(128)[None, :]
    same = (s // 64) == (t // 64)
    g = -1.0 / 16.0
    c["tri"] = np.stack([np.where(same & (s <= t), g, 0.0), np.where(same & (s >= t), g, 0.0),
                         np.where(same & (s > t), g, 0.0), np.where(same & (s < t), g, 0.0)], 1).astype(np.float32)
    c["gmask"] = np.stack([np.where(same & (s <= t), 1.0, 0.0), np.where(same & (s >= t), 1.0, 0.0)], 1).astype(bf)
    c["wmask"] = np.stack([np.where(s >= t, 1.0, 0.0), np.where(s <= t, 1.0, 0.0)], 1).astype(bf)
    d = np.arange(128) % 64
    partner = np.where((d % 32) < 16, np.arange(128) + 16, np.arange(128) - 16)
    perm = np.zeros((128, 128), np.float32)
    perm[partner, np.arange(128)] = 1.0
    c["perm"] = perm.astype(bf)
    inv_freq = 10000.0 ** (-np.arange(0, 32, 2, dtype=np.float32) / 32.0)
    pos = np.arange(L)
    row = (pos // 64).astype(np.float32)
    col = (pos % 64).astype(np.float32)
    ang = np.where((d % 64 < 32)[:, None], row[None, :] * inv_freq[d % 16][:, None], col[None, :] * inv_freq[d % 16][:, None]).astype(np.float32)
    cos = np.ones((128, T), np.float32)
    sin = np.zeros((128, T), np.float32)
    cos[:, :L] = np.cos(ang)
    sgn = np.where((d % 32) < 16, -1.0, 1.0)[:, None]
    sin[:, :L] = np.sin(ang) * sgn
    c["rope"] = np.stack([cos, sin], 1).astype(np.float32)
    j = np.arange(128)
    ph = 2.0 * np.pi * np.outer(j, j) / 128.0
    for nm, n in (("cs_lat", L), ("cs_ctx", CTX)):
        sc = 1.0 / math.sqrt(n * 128.0)
        c[nm] = np.concatenate([np.cos(ph) * sc, -np.sin(ph) * sc], 1).astype(bf)
    for nm, n in (("dft_lat", L), ("dft_ctx", CTX)):
        k = np.arange(n, dtype=np.int64)
        m = (np.outer(k, k) % n).astype(np.float64) * (2.0 * np.pi / n)
        c[nm] = np.stack([np.cos(m), np.sin(m)], 0).astype(bf)
    sel = np.zeros((2, 2, 128), np.float32)
    sel[0, 0, :] = 1.0
    sel[1, 1, :] = 1.0
    c["sel2"] = sel
    return c


WNAMES = ["w_ada", "b_ada", "g_mix", "g_ffn", "w_in", "w_decay_down", "w_decay_up", "b_decay", "g_gla", "sink",
          "w_pa", "w_pb", "w_pc", "w_out", "w_router_group", "w_router_expert", "w1", "w3", "w2", "g_final"]


def build_program(L, depth, debug=False, stop_after=None):
    assert L % 512 == 0
    T = L + CTX
    NTL = L // 128
    NT = T // 128
    nc = bass.Bass("TRN2", target_bir_lowering=False)
    S = Sched(nc)

    def din(name, shape, dt=F32):
        return nc.dram_tensor(name, list(shape), dt, kind="ExternalInput").ap()

    def dscr(name, shape, dt):
        return nc.dram_tensor(name, list(shape), dt, kind="ExternalOutput" if debug else "Internal").ap()

    x_in = din("x", [L, D])
    ctx_in = din("ctx", [CTX, D])
    cvT = din("cvT", [128, 8, 2])
    w_ada = din("w_ada", [depth, D, 6 * D])
    b_ada = din("b_ada", [depth, 6 * D])
    g_mix = din("g_mix", [depth, D])
    g_ffn = din("g_ffn", [depth, D])
    w_in = din("w_in", [depth, D, IN_COLS])
    w_dd = din("w_decay_down", [depth, 2, D, 16])
    w_du = din("w_decay_up", [depth, 2, 16, 256])
    b_dec = din("b_decay", [depth, 2, 256])
    g_gla = din("g_gla", [depth, 128])
    sink = din("sink", [depth, 8])
    w_pa = din("w_pa", [depth, 512, D])
    w_pb = din("w_pb", [depth, 512, D])
    w_pc = din("w_pc", [depth, 512, D])
    w_out = din("w_out", [depth, D, D])
    w_rg = din("w_router_group", [depth, D, 4])
    w_re = din("w_router_expert", [depth, D, 16])
    w1 = din("w1", [depth, 16, D, 512])
    w3 = din("w3", [depth, 16, D, 512])
    w2 = din("w2", [depth, 16, 512, D])
    g_final = din("g_final", [D])
    c_identb = din("identb", [128, 128], BF16)
    c_identf = din("identf", [128, 128])
    c_tri = din("tri", [128, 4, 128])
    c_gmask = din("gmask", [128, 2, 128], BF16)
    c_wmask = din("wmask", [128, 2, 128], BF16)
    c_perm = din("perm", [128, 128], BF16)
    c_rope = din("rope", [128, 2, T])
    c_cs_lat = din("cs_lat", [128, 256], BF16)
    c_cs_ctx = din("cs_ctx", [128, 256], BF16)
    c_dft_lat = din("dft_lat", [2, L, L], BF16)
    c_dft_ctx = din("dft_ctx", [2, CTX, CTX], BF16)
    c_sel2 = din("sel2", [2, 2, 128])
    out_ap = nc.dram_tensor("out", [L, D], F32, kind="ExternalOutput").ap()

    X = dscr("X", [T, D], F32)
    MODS = dscr("MODS", [depth, 2, 6 * D], F32)
    QTa = dscr("QTa", [256, T], BF16)
    KTa = dscr("KTa", [256, T], BF16)
    Ka = dscr("Ka", [T, 256], BF16)
    Va = dscr("Va", [T, 512], BF16)
    Ra = dscr("Ra", [T, 512], BF16)
    LA = dscr("LA", [T, 512], F32)
    KTc = dscr("KTc", [128, T], BF16)
    Vc = dscr("Vc", [T, 128], BF16)
    QTc = dscr("QTc", [512, T], BF16)
    UT = dscr("UT", [512, T], BF16)
    GT = dscr("GT", [3072, T], BF16)
    OF = dscr("OF", [T, 512], F32)
    OB = dscr("OB", [T, 512], F32)
    YAT = dscr("YAT", [512, T], BF16)
    YBT = dscr("YBT", [512, T], BF16)
    YCT = dscr("YCT", [512, T], BF16)
    H2T = dscr("H2T", [D, T], BF16)
    GATES = dscr("GATES", [T, 16], F32)
    W1B = nc.dram_tensor("W1B", [depth, 16, D, 512], BF16, kind="Internal").ap()
    W3B = nc.dram_tensor("W3B", [depth, 16, D, 512], BF16, kind="Internal").ap()
    W2B = nc.dram_tensor("W2B", [depth, 16, 512, D], BF16, kind="Internal").ap()

    st = ExitStack()
    A = Arena(nc, st)
    PS = [st.enter_context(nc.psum_tensor(f"ps{i}", [128, 512], F32)) for i in range(8)]

    def _fs(ap):
        n = 1
        for d in ap.shape[1:]:
            n *= int(d)
        return n

    _RATE = {"dve": 0.96e3, "act": 1.2e3, "pool": 0.6e3}

    def _ed(eng, ap):
        return 0.12 + _fs(ap) / _RATE[eng]

    def mm(out, lhsT, rhs, start, stop, rd, wr):
        f32 = lhsT.dtype == F32
        S.op("pe", lambda e: e.matmul(out, lhsT=lhsT, rhs=rhs, start=start, stop=stop), rd, wr,
             dur=0.03 + _fs(out) * (4.0 if f32 else 1.0) / 2.4e3)

    def tr(out, in_, ident, rd, wr):
        S.op("pe", lambda e: e.transpose(out=out, in_=in_, identity=ident), rd, wr, dur=0.03 + _fs(out) / 2.4e3)

    def act(out, in_, func, rd, wr, **kw):
        S.op("act", lambda e: e.activation(out=out, in_=in_, func=func, **kw), rd, wr, dur=_ed("act", out) + 0.1)

    def tt(eng, out, in0, in1, op, rd, wr):
        S.op(eng, lambda e: e.tensor_tensor(out=out, in0=in0, in1=in1, op=op), rd, wr, dur=_ed(eng, out))

    def ts(eng, out, in0, s1, s2, op0, op1, rd, wr):
        if s2 is None:
            S.op(eng, lambda e: e.tensor_scalar(out=out, in0=in0, scalar1=s1, scalar2=None, op0=op0), rd, wr, dur=_ed(eng, out))
        else:
            S.op(eng, lambda e: e.tensor_scalar(out=out, in0=in0, scalar1=s1, scalar2=s2, op0=op0, op1=op1), rd, wr, dur=_ed(eng, out))

    def stt(eng, out, in0, scalar, in1, op0, op1, rd, wr):
        S.op(eng, lambda e: e.scalar_tensor_tensor(out=out, in0=in0, scalar=scalar, in1=in1, op0=op0, op1=op1), rd, wr, dur=_ed(eng, out))

    def cp(eng, out, in_, rd, wr):
        if eng == "act":
            S.op("act", lambda e: e.copy(out=out, in_=in_), rd, wr, dur=_ed("act", out))
        else:
            S.op(eng, lambda e: e.tensor_copy(out=out, in_=in_), rd, wr, dur=_ed(eng, out))

    def amul(out, in_, mul, rd, wr):
        S.op("act", lambda e: e.mul(out=out, in_=in_, mul=mul), rd, wr, dur=_ed("act", out))

    def memset(eng, ap, val, wr):
        S.op(eng, lambda e: e.memset(ap, val), (), wr, dur=_ed(eng, ap))

    def recip(out, in_, rd, wr):
        S.op("dve", lambda e: e.reciprocal(out=out, in_=in_), rd, wr, dur=_ed("dve", out))

    def bcast_row(ap_row, n):
        return ap_row.partition_broadcast(128)

    identb = A.tile("identb", [128, 128], BF16)
    identf = A.tile("identf", [128, 128], F32)
    tri = A.tile("tri", [128, 4, 128], F32)
    gmask = A.tile("gmask", [128, 2, 128], BF16)
    wmask = A.tile("wmask", [128, 2, 128], BF16)
    perm = A.tile("perm", [128, 128], BF16)
    cs_lat = A.tile("cs_lat", [128, 256], BF16)
    cs_ctx = A.tile("cs_ctx", [128, 256], BF16)
    sel2 = A.tile("sel2", [2, 2, 128], F32)
    for tl, src in ((identb, c_identb), (identf, c_identf), (tri, c_tri), (gmask, c_gmask), (wmask, c_wmask),
                    (perm, c_perm), (cs_lat, c_cs_lat), (cs_ctx, c_cs_ctx), (sel2, c_sel2)):
        S.dma("sp", tl[:], src, writes=[tl])

    S.dma("sp", X[0:L, :], x_in, writes=[("X", i) for i in range(NTL)])
    S.dma("sp", X[L:T, :], ctx_in, writes=[("X", i) for i in range(NTL, NT)])

    def blocks_of(width, lo, hi):
        r = []
        p = lo
        while p < hi:
            w = min(width, hi - p)
            r.append((p, w))
            p += w
        return r

    def xkeys(p0, w):
        return [("X", i) for i in range(p0 // 128, (p0 + w) // 128)]

    def rms_scale(ss, rstd, n, rd, wr):
        act(rstd, ss, AF.Sqrt, rd, wr, scale=1.0 / n, bias=EPS)
        recip(rstd, rstd, wr, wr)

    def phase_M(l):
        cT = A.tile("cT", [128, 8, 2], F32)
        cTb = A.tile("cTb", [128, 8, 2], BF16)
        bch = [A.tile(f"bch{i}", [2, 512], F32) for i in range(2)]
        mch = [A.tile(f"mch{i}", [2, 512], F32) for i in range(2)]
        wa = [A.tile(f"wa{i}", [128, 8, 512], BF16) for i in range(2)]
        S.dma("sp", cT[:], cvT, writes=[cT])
        act(cTb[:], cT[:], AF.Silu, [cT], [cTb])
        for cb in range(12):
            w = wa[cb % 2]
            cs_ = slice(cb * 512, (cb + 1) * 512)
            S.dma("pool", w[:], w_ada[l, :, cs_].rearrange("(k p) n -> p k n", p=128), writes=[w])
            bc = bch[cb % 2]
            S.dma("sp", bc[0:1, :], b_ada[l:l + 1, cs_], writes=[(bc, 0)])
            S.dma("sp", bc[1:2, :], b_ada[l:l + 1, cs_], writes=[(bc, 1)])
            pb = PS[6 + cb % 2]
            for kc in range(8):
                mm(pb[0:2, :], cTb[:, kc, :], w[:, kc, :], kc == 0, kc == 7, [cTb, w], [pb])
            mc = mch[cb % 2]
            tt("dve", mc[:], pb[0:2, :], bc[:], ALU.add, [pb, (bc, 0), (bc, 1)], [mc])
            S.dma("sp", MODS[l, :, cs_], mc[:], reads=[mc], writes=[("MODS", l, cb)])
        for e in range(16):
            S.dma("pool", W1B[l, e], w1[l, e], writes=[("W1B", l, e)])
            S.dma("pool", W3B[l, e], w3[l, e], writes=[("W3B", l, e)])
            S.dma("pool", W2B[l, e], w2[l, e], writes=[("W2B", l, e)])

    def load_mod_bcast(dst, l, r, j):
        S.dma("sp", dst[:], bcast_row(MODS[l, r, j * D:(j + 1) * D], D), reads=[("MODS", l, 2 * j), ("MODS", l, 2 * j + 1)], writes=[dst])

    def phase_A(l):
        A.push()
        win = A.tile("win", [128, 8, IN_COLS], BF16)
        S.dma("pool", win[:, 0:4, :], w_in[l, 0:512, :].rearrange("(k p) n -> p k n", p=128), writes=[(win, 0)])
        S.dma("pool", win[:, 4:8, :], w_in[l, 512:1024, :].rearrange("(k p) n -> p k n", p=128), writes=[(win, 1)])
        WIN = [(win, 0), (win, 1)]
        wd = A.tile("wd", [128, 8, 32], BF16)
        for dr in range(2):
            S.dma("pool", wd[:, :, dr * 16:(dr + 1) * 16], w_dd[l, dr].rearrange("(k p) n -> p k n", p=128), writes=[(wd, dr)])
        WD = [(wd, 0), (wd, 1)]
        wu = A.tile("wu", [33, 512], BF16)
        memset("dve", wu[:], 0.0, [wu])
        wu_f = A.tile("wu_f", [33, 512], F32)
        memset("dve", wu_f[:], 0.0, [wu_f])
        S.dma("sp", wu_f[0:16, 0:256], w_du[l, 0], reads=[wu_f], writes=[wu_f])
        S.dma("sp", wu_f[16:32, 256:512], w_du[l, 1], reads=[wu_f], writes=[wu_f])
        S.dma("sp", wu_f[32:33, :], b_dec[l:l + 1].rearrange("o a b -> o (a b)"), reads=[wu_f], writes=[wu_f])
        cp("dve", wu[:], wu_f[:], [wu_f], [wu])
        gm = A.tile("gm", [128, D], F32)
        S.dma("sp", gm[:], bcast_row(g_mix[l], D), writes=[gm])
        A1, B1 = [], []
        for r in range(2):
            a1 = A.tile(f"A1_{r}", [128, D], F32)
            b1 = A.tile(f"B1_{r}", [128, D], F32)
            load_mod_bcast(a1, l, r, 1)
            load_mod_bcast(b1, l, r, 0)
            stt("dve", a1[:], a1[:], 1.0, gm[:], ALU.add, ALU.mult, [a1, gm], [a1])
            A1.append(a1)
            B1.append(b1)
        NB = 2
        xt = [A.tile(f"xt{i}", [128, D], F32) for i in range(NB)]
        junk = A.tile("junkA", [128, D], F32)
        ss = [A.tile(f"ss{i}", [128, 2], F32) for i in range(NB)]
        hn = [A.tile(f"hn{i}", [128, D], F32) for i in range(NB)]
        hb = [A.tile(f"hb{i}", [128, D], BF16) for i in range(NB)]
        hT = [A.tile(f"hT{i}", [128, 8, 512], BF16) for i in range(2)]
        t1T = [A.tile(f"t1T{i}", [33, 512], BF16) for i in range(2)]
        for i in range(2):
            memset("dve", t1T[i][:], 1.0, [t1T[i]])
        NO = 4
        ob = [A.tile(f"ob{i}", [128, 512], BF16) for i in range(NO)]
        of = [A.tile(f"of{i}", [128, 512], F32) for i in range(NO)]
        rp = [A.tile(f"rp{i}", [128, 2, 512], F32) for i in range(2)]
        ra = [A.tile(f"ra{i}", [128, 512], F32) for i in range(2)]
        rb = [A.tile(f"rb{i}", [128, 512], F32) for i in range(2)]
        cnt = {"ob": 0, "of": 0, "ps": 0, "r": 0}

        def nxt(k, n):
            v = cnt[k] % n
            cnt[k] += 1
            return v

        def psb():
            return PS[nxt("ps", 6)]

        blks = blocks_of(512, 0, L) + blocks_of(512, L, T)

        def prep(bi):
            p0, bw = blks[bi]
            r = 0 if p0 < L else 1
            nt = bw // 128
            h_T = hT[bi % 2]
            for j in range(nt):
                tix = p0 // 128 + j
                u = (bi * 4 + j) % NB
                S.dma("sp", xt[u][:], X[tix * 128:(tix + 1) * 128, :], reads=[("X", tix)], writes=[xt[u]])
                act(junk[:], xt[u][:], AF.Square, [xt[u]], [junk, (ss[u], 0)], accum_out=ss[u][:, 0:1])
                rms_scale(ss[u][:, 0:1], ss[u][:, 1:2], float(D), [(ss[u], 0)], [(ss[u], 1)])
                stt("dve", hn[u][:], xt[u][:], ss[u][:, 1:2], A1[r][:], ALU.mult, ALU.mult, [xt[u], (ss[u], 1), A1[r]], [hn[u]])
                tt("pool", hb[u][:], hn[u][:], B1[r][:], ALU.add, [hn[u], B1[r]], [hb[u]])
                pb = psb()
                pbb = pb[:].bitcast(BF16)
                for kc in range(8):
                    tr(pbb[:, kc * 128:(kc + 1) * 128], hb[u][:, kc * 128:(kc + 1) * 128], identb[:], [hb[u], identb], [pb])
                cp("act", h_T[:, :, j * 128:(j + 1) * 128], pbb[:].rearrange("p (k t) -> p k t", k=8), [pb], [(h_T, j)])

        def part1(bi):
            p0, bw = blks[bi]
            r = 0 if p0 < L else 1
            nt = bw // 128
            h_T = hT[bi % 2]
            HT = [(h_T, j) for j in range(nt)]
            tsl = slice(p0, p0 + bw)
            rpt = rp[bi % 2]
            S.dma("sp", rpt[:, :, 0:bw], c_rope[:, :, p0:p0 + bw], writes=[rpt])

            def fm(col0, dst_cb):
                pb = psb()
                for kc in range(8):
                    mm(pb[:, 0:bw], win[:, kc, col0:col0 + 128], h_T[:, kc, 0:bw], kc == 0, kc == 7, WIN + HT, [pb])
                dst_cb(pb)

            def fm_plain(dram, row0, scale, key):
                def cb(pb):
                    o = ob[nxt("ob", NO)]
                    if scale == 1.0:
                        cp("act", o[:, 0:bw], pb[:, 0:bw], [pb], [o])
                    else:
                        amul(o[:, 0:bw], pb[:, 0:bw], scale, [pb], [o])
                    S.dma("sp", dram[row0:row0 + 128, tsl], o[:, 0:bw], reads=[o], writes=[(key, row0, bi)])
                return cb

            def fm_rope(dram, row0, scale, key):
                def cb(pb):
                    o = ob[nxt("ob", NO)]
                    amul(o[:, 0:bw], pb[:, 0:bw], scale, [pb], [o])
                    p2 = psb()
                    mm(p2[:, 0:bw], perm[:], o[:, 0:bw], True, True, [perm, o], [p2])
                    k = nxt("r", 2)
                    tt("dve", ra[k][:, 0:bw], o[:, 0:bw], rpt[:, 0, 0:bw], ALU.mult, [o, rpt], [ra[k]])
                    tt("dve", rb[k][:, 0:bw], p2[:, 0:bw], rpt[:, 1, 0:bw], ALU.mult, [p2, rpt], [rb[k]])
                    o2 = ob[nxt("ob", NO)]
                    tt("pool", o2[:, 0:bw], ra[k][:, 0:bw], rb[k][:, 0:bw], ALU.add, [ra[k], rb[k]], [o2])
                    S.dma("sp", dram[row0:row0 + 128, tsl], o2[:, 0:bw], reads=[o2], writes=[(key, row0, bi)])
                return cb

            def fm_sig(row0):
                def cb(pb):
                    o = ob[nxt("ob", NO)]
                    act(o[:, 0:bw], pb[:, 0:bw], AF.Sigmoid, [pb], [o])
                    S.dma("sp", GT[row0:row0 + 128, tsl], o[:, 0:bw], reads=[o], writes=[("GT", row0, bi)])
                return cb

            for c2 in range(2):
                fm(C_KA + c2 * 128, fm_plain(KTa, c2 * 128, 1.0, "KTa"))
            for c2 in range(2):
                fm(C_QA + c2 * 128, fm_plain(QTa, c2 * 128, 0.125, "QTa"))
            fm(C_KC, fm_rope(KTc, 0, 1.0, "KTc"))
            for c4 in range(4):
                fm(C_QC + c4 * 128, fm_rope(QTc, c4 * 128, 0.125, "QTc"))
            for c4 in range(4):
                fm(C_UB + c4 * 128, fm_plain(UT, c4 * 128, 1.0, "UT"))
            for c24 in range(24):
                fm(C_G + c24 * 128, fm_sig(c24 * 128))

        def part2(bi):
            p0, bw = blks[bi]
            r = 0 if p0 < L else 1
            nt = bw // 128
            h_T = hT[bi % 2]
            HT = [(h_T, j) for j in range(nt)]
            tsl = slice(p0, p0 + bw)
            pb = psb()
            for kc in range(8):
                mm(pb[0:32, 0:bw], wd[:, kc, :], h_T[:, kc, 0:bw], kc == 0, kc == 7, WD + HT, [pb])
            t1 = t1T[bi % 2]
            cp("act", t1[0:32, 0:bw], pb[0:32, 0:bw], [pb], [t1])
            for j in range(nt):
                tix = p0 // 128 + j
                rows = slice(tix * 128, (tix + 1) * 128)
                lhs = lambda kc: h_T[:, kc, j * 128:(j + 1) * 128]
                pb = psb()
                for kc in range(8):
                    mm(pb[:], lhs(kc), win[:, kc, 0:512], kc == 0, kc == 7, WIN + HT, [pb])
                o = ob[nxt("ob", NO)]
                cp("act", o[:], pb[:], [pb], [o])
                S.dma("sp", Ka[rows, :], o[:, 0:256], reads=[o], writes=[("Ka", tix)])
                S.dma("sp", Va[rows, 0:256], o[:, 256:512], reads=[o], writes=[("Va0", tix)])
                pb = psb()
                for kc in range(8):
                    mm(pb[:], lhs(kc), win[:, kc, 512:1024], kc == 0, kc == 7, WIN + HT, [pb])
                o = ob[nxt("ob", NO)]
                cp("dve", o[:], pb[:], [pb], [o])
                S.dma("sp", Va[rows, 256:512], o[:, 0:256], reads=[o], writes=[("Va1", tix)])
                S.dma("sp", Vc[rows, :], o[:, 384:512], reads=[o], writes=[("Vc", tix)])
                pb = psb()
                for kc in range(8):
                    mm(pb[:], lhs(kc), win[:, kc, C_RA:C_RA + 512], kc == 0, kc == 7, WIN + HT, [pb])
                o = ob[nxt("ob", NO)]
                cp("act", o[:], pb[:], [pb], [o])
                S.dma("sp", Ra[rows, :], o[:], reads=[o], writes=[("Ra", tix)])
                pb = psb()
                mm(pb[:], t1[0:33, j * 128:(j + 1) * 128], wu[:], True, True, [t1, wu], [pb])
                f = of[nxt("of", NO)]
                act(f[:], pb[:], AF.Exp, [pb], [f], scale=-1.0)
                act(f[:], f[:], AF.Ln, [f], [f], bias=1.0, scale=1.0)
                S.dma("sp", LA[rows, :], f[:], reads=[f], writes=[("LA", tix)])

        prep(0)
        for bi in range(len(blks)):
            part1(bi)
            if bi + 1 < len(blks):
                prep(bi + 1)
            part2(bi)
        A.pop()

    def phase_B(l):
        A.push()
        S32 = A.tile("S32", [128, 4, 128], F32)
        Sbf = A.tile("Sbf", [128, 4, 128], BF16)
        tmpS = A.tile("tmpS", [128, 4, 128], F32)
        memset("dve", S32[:], 0.0, [S32])
        memset("dve", Sbf[:], 0.0, [Sbf])
        Sbf2 = [Sbf, A.tile("Sbf_b", [128, 4, 128], BF16)]
        qT2 = [A.tile(f"qT2{u}", [128, 4, 128], BF16) for u in range(2)]
        kT2 = [A.tile(f"kT2{u}", [128, 4, 128], BF16) for u in range(2)]
        kx = [[A.tile(f"kx{d}{u}", [128, 256], BF16) for u in range(2)] for d in range(2)]
        vx = [[A.tile(f"vx{d}{u}", [128, 512], BF16) for u in range(2)] for d in range(2)]
        lax = [[A.tile(f"lax{d}{u}", [128, 256], F32) for u in range(2)] for d in range(2)]
        E1 = [A.tile(f"E1{u}", [128, 4, 128], F32) for u in range(2)]
        E2 = [A.tile(f"E2{u}", [128, 4, 128], F32) for u in range(2)]
        ED = [A.tile(f"ED{u}", [128, 512], F32) for u in range(2)]
        qe = [A.tile(f"qe{u}", [128, 4, 128], BF16) for u in range(2)]
        ke = [A.tile(f"ke{u}", [128, 4, 128], BF16) for u in range(2)]
        kd = [[A.tile(f"kd{d}{u}", [128, 256], BF16) for u in range(2)] for d in range(2)]
        scT = [[A.tile(f"scT{d}{u}", [128, 4, 128], BF16) for u in range(2)] for d in range(2)]
        EL = [A.tile(f"EL{u}", [128, 2, 4], F32) for u in range(2)]
        osb = [[A.tile(f"osb{d}{u}", [128, 512], F32) for u in range(2)] for d in range(2)]
        Pc, Pd, Pst = PS[0], PS[1], PS[6]
        Ps = [PS[2], PS[2]]
        Po = [PS[3], PS[4]]
        Pi = [PS[5], PS[7]]
        order = [[NTL, NTL + 1] + list(range(NTL)), [NTL + 1, NTL] + list(range(NTL - 1, -1, -1))]
        ODR = [OF, OB]

        def v3(ap):
            return ap.rearrange("p (h t) -> p h t", h=4)

        def prelude(i):
            u = i % 2
            tx = [order[0][i], order[1][i]]
            for d in range(2):
                cols = slice(tx[d] * 128, (tx[d] + 1) * 128)
                rows = cols
                S.dma("sp", qT2[u][d * 64:(d + 1) * 64, :, :], QTa[:, cols].rearrange("(h k) t -> k h t", h=4), writes=[(qT2[u], d)])
                S.dma("sp", kT2[u][d * 64:(d + 1) * 64, :, :], KTa[:, cols].rearrange("(h k) t -> k h t", h=4), writes=[(kT2[u], d)])
                S.dma("sp", kx[d][u][:], Ka[rows, :], writes=[kx[d][u]])
                S.dma("sp", vx[d][u][:], Va[rows, :], writes=[vx[d][u]])
                S.dma("sp", lax[d][u][:], LA[rows, d * 256:(d + 1) * 256], writes=[lax[d][u]])
            for d in range(2):
                for h in range(4):
                    mm(Pc[d * 64:(d + 1) * 64, h * 128:(h + 1) * 128], lax[d][u][:, h * 64:(h + 1) * 64], tri[:, d, :], True, True,
                       [lax[d][u], tri], [Pc])
            for d in range(2):
                mm(Pd[:, d * 256:(d + 1) * 256], tri[:, 2 + d, :], lax[d][u][:], True, True, [lax[d][u], tri], [Pd])
            act(E1[u][:], v3(Pc[:]), AF.Exp, [Pc], [E1[u]])
            act(E2[u][:], v3(Pc[:]), AF.Exp, [Pc], [E2[u]], scale=-1.0)
            act(ED[u][:], Pd[:], AF.Exp, [Pd], [ED[u]])
            tt("dve", qe[u][:], qT2[u][:], E1[u][:], ALU.mult, [(qT2[u], 0), (qT2[u], 1), E1[u]], [qe[u]])
            tt("pool", ke[u][:], kT2[u][:], E2[u][:], ALU.mult, [(kT2[u], 0), (kT2[u], 1), E2[u]], [ke[u]])
            tt("dve", kd[0][u][:], kx[0][u][:], ED[u][:, 0:256], ALU.mult, [kx[0][u], ED[u]], [kd[0][u]])
            tt("pool", kd[1][u][:], kx[1][u][:], ED[u][:, 256:512], ALU.mult, [kx[1][u], ED[u]], [kd[1][u]])
            for ci in range(2):
                cf, cb = ci, 1 - ci
                cp("pool", EL[u][0:64, ci, :], E1[u][0:64, :, cf * 64 + 63], [E1[u]], [(EL[u], ci, 0)])
                cp("pool", EL[u][64:128, ci, :], E1[u][64:128, :, cb * 64], [E1[u]], [(EL[u], ci, 1)])
            for d in range(2):
                for h in range(4):
                    for cx in range(2):
                        cs_ = slice(cx * 64, (cx + 1) * 64)
                        mm(Ps[d][cs_, h * 128 + cx * 64:h * 128 + (cx + 1) * 64], ke[u][d * 64:(d + 1) * 64, h, cs_], qe[u][d * 64:(d + 1) * 64, h, cs_],
                           True, True, [ke[u], qe[u]], [Ps[d]])
                tt("dve", scT[d][u][:], v3(Ps[d][:]), gmask[:, d, :].unsqueeze(1).to_broadcast([128, 4, 128]), ALU.mult,
                   [Ps[d], gmask], [scT[d][u]])
        def chain(i):
            u = i % 2
            tx = [order[0][i], order[1][i]]
            for ci in range(2):
                sb_in = Sbf2[(2 * i + ci) % 2]
                sb_out = Sbf2[(2 * i + ci + 1) % 2]
                for d in range(2):
                    cx = ci if d == 0 else 1 - ci
                    cs_ = slice(cx * 64, (cx + 1) * 64)
                    for h in range(4):
                        mm(Pst[d * 64:(d + 1) * 64, h * 128:(h + 1) * 128], kd[d][u][cs_, h * 64:(h + 1) * 64], vx[d][u][cs_, h * 128:(h + 1) * 128],
                           True, True, [kd[d][u], vx[d][u]], [Pst])
                tt("dve", tmpS[:], S32[:], EL[u][:, ci, :].unsqueeze(2).to_broadcast([128, 4, 128]), ALU.mult,
                   [S32, (EL[u], ci, 0), (EL[u], ci, 1)], [tmpS])
                tt("dve", S32[:], tmpS[:], v3(Pst[:]), ALU.add, [tmpS, Pst], [S32])
                cp("act", sb_out[:], S32[:], [S32], [sb_out])
                for d in range(2):
                    cx = ci if d == 0 else 1 - ci
                    cs_ = slice(cx * 64, (cx + 1) * 64)
                    ds_ = slice(d * 64, (d + 1) * 64)
                    for h in range(4):
                        hs = slice(h * 128, (h + 1) * 128)
                        mm(Po[d][cs_, hs], scT[d][u][cs_, h, cs_], vx[d][u][cs_, hs], True, True, [scT[d][u], vx[d][u]], [Po[d]])
                    for h in range(4):
                        hs = slice(h * 128, (h + 1) * 128)
                        mm(Pi[d][cs_, hs], qe[u][ds_, h, cs_], sb_in[ds_, h, :], True, True, [qe[u], sb_in], [Pi[d]])
            for d in range(2):
                cp("act", osb[d][u][:], Po[d][:], [Po[d]], [osb[d][u]])
                tt("dve", osb[d][u][:], Pi[d][:], osb[d][u][:], ALU.add, [Pi[d], osb[d][u]], [osb[d][u]])
                S.dma("sp", ODR[d][tx[d] * 128:(tx[d] + 1) * 128, :], osb[d][u][:], reads=[osb[d][u]], writes=[("O", d, tx[d])])
        prelude(0)
        for i in range(NT):
            if i + 1 < NT:
                prelude(i + 1)
            chain(i)
        GG = A.tile("GG", [128, 128], F32)
        S.dma("sp", GG[:], bcast_row(g_gla[l], 128), writes=[GG])
        o1 = [A.tile(f"o1{u}", [128, 512], F32) for u in range(2)]
        o2 = [A.tile(f"o2{u}", [128, 512], F32) for u in range(2)]
        rr = [A.tile(f"rr{u}", [128, 512], BF16) for u in range(2)]
        osum = [A.tile(f"osum{u}", [128, 4, 128], F32) for u in range(2)]
        junk = A.tile("junkB", [128, 128], F32)
        ssg = [A.tile(f"ssg{u}", [128, 8], F32) for u in range(2)]
        sr = [A.tile(f"sr{u}", [128, 512], F32) for u in range(2)]
        t1 = [A.tile(f"t1{u}", [128, 4, 128], F32) for u in range(2)]
        yb_ = [A.tile(f"yb{u}", [128, 512], BF16) for u in range(2)]
        yT = [A.tile(f"yT{u}", [128, 4, 128], BF16) for u in range(2)]
        for t in range(NT):
            u = t % 2
            rows = slice(t * 128, (t + 1) * 128)
            S.dma("sp", o1[u][:], OF[rows, :], reads=[("O", 0, t)], writes=[o1[u]])
            S.dma("sp", o2[u][:], OB[rows, :], reads=[("O", 1, t)], writes=[o2[u]])
            S.dma("sp", rr[u][:], Ra[rows, :], writes=[rr[u]])
            tt("pool", osum[u][:], v3(o1[u][:]), v3(o2[u][:]), ALU.add, [o1[u], o2[u]], [osum[u]])
            for h in range(4):
                act(junk[:], osum[u][:, h, :], AF.Square, [osum[u]], [junk, (ssg[u], h)], accum_out=ssg[u][:, h:h + 1])
            rms_scale(ssg[u][:, 0:4], ssg[u][:, 4:8], 128.0, [(ssg[u], h) for h in range(4)], [(ssg[u], 9)])
            tt("dve", t1[u][:], osum[u][:], ssg[u][:, 4:8].unsqueeze(2).to_broadcast([128, 4, 128]), ALU.mult, [osum[u], (ssg[u], 9)], [t1[u]])
            act(sr[u][:], rr[u][:], AF.Silu, [rr[u]], [sr[u]])
            tt("pool", t1[u][:], t1[u][:], GG[:].unsqueeze(1).to_broadcast([128, 4, 128]), ALU.mult, [t1[u], GG], [t1[u]])
            tt("dve", yb_[u][:], t1[u][:].rearrange("p h t -> p (h t)"), sr[u][:], ALU.mult, [t1[u], sr[u]], [yb_[u]])
            pb = PS[t % 2]
            pbb = pb[:].bitcast(BF16)
            for c4 in range(4):
                tr(pbb[:, c4 * 128:(c4 + 1) * 128], yb_[u][:, c4 * 128:(c4 + 1) * 128], identb[:], [yb_[u], identb], [pb])
            cp("act", yT[u][:], pbb[:, 0:512].rearrange("p (c t) -> p c t", c=4), [pb], [yT[u]])
            S.dma("sp", YAT[:, rows].rearrange("(c p) t -> p c t", p=128), yT[u][:], reads=[yT[u]], writes=[("YAT", t)])
        A.pop()

    def phase_C(l, last):
        A.push()
        for which, n, t0, cs, dft in (("lat", L, 0, cs_lat, c_dft_lat), ("ctx", CTX, L, cs_ctx, c_dft_ctx)):
            if which == "ctx" and last:
                continue
            ntl = n // 128
            AB = A.tile("AB", [128, ntl, 4, 256], BF16)
            uT = [A.tile(f"uT{u}", [128, 4, 128], BF16) for u in range(2)]
            for t in range(ntl):
                u = t % 2
                S.dma("sp", uT[u][:], UT[:, t0 + t * 128:t0 + (t + 1) * 128].rearrange("(g c) t -> c g t", g=4), writes=[uT[u]])
                P0, P1 = PS[(t % 2) * 2], PS[(t % 2) * 2 + 1]
                for g in range(4):
                    pg = P0 if g < 2 else P1
                    mm(pg[:, (g % 2) * 256:(g % 2 + 1) * 256], uT[u][:, g, :], cs[:], True, True, [uT[u], cs], [pg])
                cp("act", AB[:, t, 0:2, :], P0[:].rearrange("p (g c) -> p g c", g=2), [P0], [(AB, t, 0)])
                cp("dve", AB[:, t, 2:4, :], P1[:].rearrange("p (g c) -> p g c", g=2), [P1], [(AB, t, 1)])
            ABK = [(AB, t, q) for t in range(ntl) for q in range(2)]
            GRP = min(8, ntl)
            pbw = min(512, n)
            cl = [A.tile(f"cl{u}", [128, GRP, pbw], BF16) for u in range(3)]
            sl = [A.tile(f"sl{u}", [128, GRP, pbw], BF16) for u in range(3)]
            ybt = [A.tile(f"ybt{u}", [128, 4, pbw], BF16) for u in range(2)]
            k3 = 0
            for pb_ in range(n // pbw):
                Pg = [PS[4 + g] for g in range(4)]
                for lq in range(ntl // GRP):
                    c_, s_ = cl[k3 % 3], sl[k3 % 3]
                    k3 += 1
                    rws = slice(lq * GRP * 128, (lq + 1) * GRP * 128)
                    cls = slice(pb_ * pbw, (pb_ + 1) * pbw)
                    S.dma("sp", c_[:], dft[0, rws, cls].rearrange("(k p) n -> p k n", p=128), writes=[c_])
                    S.dma("sp", s_[:], dft[1, rws, cls].rearrange("(k p) n -> p k n", p=128), writes=[s_])
                    for g in range(4):
                        for k in range(GRP):
                            lt = lq * GRP + k
                            mm(Pg[g][:, 0:pbw], AB[:, lt, g, 0:128], c_[:, k, :], lt == 0, False, ABK + [c_], [Pg[g]])
                            mm(Pg[g][:, 0:pbw], AB[:, lt, g, 128:256], s_[:, k, :], False, lt == ntl - 1, ABK + [s_], [Pg[g]])
                yo = ybt[pb_ % 2]
                for g in range(4):
                    cp("act" if g % 2 == 0 else "dve", yo[:, g, :], Pg[g][:, 0:pbw], [Pg[g]], [(yo, g)])
                S.dma("sp", YBT[:, t0 + pb_ * pbw:t0 + (pb_ + 1) * pbw].rearrange("(g j) t -> j g t", g=4), yo[:],
                      reads=[(yo, g) for g in range(4)], writes=[("YBT", which, pb_)])
        A.pop()

    def phase_D(l, last):
        A.push()
        kTa = A.tile("kTall", [64, 2, T], BF16)
        Vg = A.tile("Vaug", [128, NT, 2, 65], BF16)
        ES = A.tile("ES", [128, 8], F32)
        S.dma("sp", kTa[:], KTc.rearrange("(v k) t -> k v t", v=2), writes=[kTa])
        memset("pool", Vg[:], 1.0, [Vg])
        for t in range(NT):
            S.dma("sp", Vg[:, t, :, 0:64], Vc[t * 128:(t + 1) * 128, :].rearrange("p (v k) -> p v k", v=2), reads=[Vg], writes=[Vg])
        S.dma("sp", ES[:], bcast_row(sink[l], 8), writes=[ES])
        act(ES[:], ES[:], AF.Exp, [ES], [ES])
        qt = [A.tile(f"qt{u}", [64, 8 * 128], BF16) for u in range(2)]
        pT = [A.tile(f"pT{u}", [128, 512], BF16) for u in range(12)]
        den = [A.tile(f"den{u}", [128, 8], F32) for u in range(2)]
        yc = [A.tile(f"yc{u}", [128, 8, 64], BF16) for u in range(2)]
        ycT = [A.tile(f"ycT{u}", [128, 4, 128], BF16) for u in range(2)]
        nblk = NTL if last else NT
        kpc = {"kp": 0}

        def body(n):
            u = n % 2
            cols = slice(n * 128, (n + 1) * 128)
            S.dma("sp", qt[u][:].rearrange("k (h t) -> k h t", h=8), QTc[:, cols].rearrange("(h k) t -> k h t", h=8), writes=[qt[u]])
            if n < NTL:
                chunks = ([(n - 1, 0)] if n > 0 else []) + [(n, None)] + ([(n + 1, 1)] if n < NTL - 1 else []) + [(NTL, None), (NTL + 1, None)]
            else:
                chunks = [(NTL, None), (NTL + 1, None)]
            Pv = [PS[3 + u * 2], PS[4 + u * 2]]
            for v in range(2):
                pts = []
                for ci, (kt, mk) in enumerate(chunks):
                    kp = kpc["kp"]
                    pk = PS[kp % 3]
                    p_ = pT[kp % 12]
                    kpc["kp"] += 1
                    mm(pk[:], kTa[:, v, kt * 128:(kt + 1) * 128], qt[u][:, v * 512:(v + 1) * 512], True, True, [kTa, qt[u]], [pk])
                    act(p_[:], pk[:], AF.Exp, [pk], [p_])
                    if mk is not None:
                        tt("pool", p_[:].rearrange("p (h t) -> p h t", h=4), p_[:].rearrange("p (h t) -> p h t", h=4),
                           wmask[:, mk, :].unsqueeze(1).to_broadcast([128, 4, 128]), ALU.mult, [p_, wmask], [p_])
                    pts.append(p_)
                for h in range(4):
                    for ci, (kt, mk) in enumerate(chunks):
                        mm(Pv[v][:, h * 65:(h + 1) * 65], pts[ci][:, h * 128:(h + 1) * 128], Vg[:, kt, v, :], ci == 0, ci == len(chunks) - 1,
                           [pts[ci], Vg], [Pv[v]])

        def tail(n):
            u = n % 2
            cols = slice(n * 128, (n + 1) * 128)
            Pv = [PS[3 + u * 2], PS[4 + u * 2]]
            for v in range(2):
                acc = Pv[v][:, 0:260].rearrange("p (h c) -> p h c", h=4)
                tt("dve", den[u][:, v * 4:(v + 1) * 4], acc[:, :, 64], ES[:, v * 4:(v + 1) * 4], ALU.add, [Pv[v], ES], [(den[u], v)])
                recip(den[u][:, v * 4:(v + 1) * 4], den[u][:, v * 4:(v + 1) * 4], [(den[u], v)], [(den[u], v)])
                tt("dve", yc[u][:, v * 4:(v + 1) * 4, :], acc[:, :, 0:64], den[u][:, v * 4:(v + 1) * 4].unsqueeze(2).to_broadcast([128, 4, 64]),
                   ALU.mult, [Pv[v], (den[u], v)], [(yc[u], v)])
            pb = PS[7]
            pbb = pb[:].bitcast(BF16)
            ycf = yc[u][:].rearrange("p h k -> p (h k)")
            for c4 in range(4):
                tr(pbb[:, c4 * 128:(c4 + 1) * 128], ycf[:, c4 * 128:(c4 + 1) * 128], identb[:], [(yc[u], 0), (yc[u], 1), identb], [pb])
            cp("dve", ycT[u][:], pbb[:, 0:512].rearrange("p (c t) -> p c t", c=4), [pb], [ycT[u]])
            S.dma("sp", YCT[:, cols].rearrange("(c p) t -> p c t", p=128), ycT[u][:], reads=[ycT[u]], writes=[("YCT", n)])

        for n in range(nblk):
            body(n)
            if n > 0:
                tail(n - 1)
        tail(nblk - 1)
        A.pop()

    def phase_E(l, last):
        A.push()
        wp = []
        for nm, src in (("wpa", w_pa), ("wpb", w_pb), ("wpc", w_pc)):
            w = A.tile(nm, [128, 4, D], BF16)
            S.dma("pool", w[:], src[l].rearrange("(k p) n -> p k n", p=128), writes=[w])
            wp.append(w)
        wo = A.tile("wo", [128, 8, D], BF16)
        S.dma("pool", wo[:], w_out[l].rearrange("(k p) n -> p k n", p=128), writes=[wo])
        wr = A.tile("wr", [128, 8, 20], BF16)
        S.dma("pool", wr[:, :, 0:4], w_rg[l].rearrange("(k p) n -> p k n", p=128), writes=[(wr, 0)])
        S.dma("pool", wr[:, :, 4:20], w_re[l].rearrange("(k p) n -> p k n", p=128), writes=[(wr, 1)])
        gf = A.tile("gf", [128, D], F32)
        S.dma("sp", gf[:], bcast_row(g_ffn[l], D), writes=[gf])
        G1, A2, B2 = {}, {}, {}
        for r in ((0,) if last else (0, 1)):
            G1[r] = A.tile(f"G1_{r}", [128, D], F32)
            A2[r] = A.tile(f"A2_{r}", [128, D], F32)
            B2[r] = A.tile(f"B2_{r}", [128, D], F32)
            load_mod_bcast(G1[r], l, r, 2)
            load_mod_bcast(B2[r], l, r, 3)
            load_mod_bcast(A2[r], l, r, 4)
            stt("dve", A2[r][:], A2[r][:], 1.0, gf[:], ALU.add, ALU.mult, [A2[r], gf], [A2[r]])
        yin = [[A.tile(f"yin{b}{u}", [128, 4, 512], BF16) for u in range(2)] for b in range(3)]
        gin = [A.tile(f"gin{b}", [128, 8, 512], BF16) for b in range(3)]
        zz = [A.tile(f"zz{i}", [128, 512], F32) for i in range(3)]
        zT = A.tile("zT", [128, 8, 512], BF16)
        xt = [A.tile(f"xtE{u}", [128, D], F32) for u in range(2)]
        tmp = [A.tile(f"tmpE{u}", [128, D], F32) for u in range(2)]
        xn = [A.tile(f"xn{u}", [128, D], F32) for u in range(2)]
        junk = A.tile("junkE", [128, D], F32)
        ss = [A.tile(f"ssE{u}", [128, 2], F32) for u in range(2)]
        hn = [A.tile(f"hnE{u}", [128, D], F32) for u in range(2)]
        hb = [A.tile(f"hbE{u}", [128, D], BF16) for u in range(2)]
        h2T = [A.tile(f"h2T{u}", [128, 8, 512], BF16) for u in range(2)]
        RT = [dict(lg=A.tile(f"lg{u}", [128, 20], F32), s=A.tile(f"rs{u}", [128, 16], F32), oh=A.tile(f"oh{u}", [128, 4], F32),
                   eg=A.tile(f"eg{u}", [128, 4], F32), prod=A.tile(f"prod{u}", [128, 4, 4], F32), ein=A.tile(f"ein{u}", [128, 4], F32),
                   mk1=A.tile(f"mk1{u}", [128, 4], F32), e2=A.tile(f"e2{u}", [128, 4], F32), mk2=A.tile(f"mk2{u}", [128, 4], F32),
                   gi=A.tile(f"gi{u}", [128, 4], F32), gt=A.tile(f"gt{u}", [128, 4, 4], F32)) for u in range(2)]
        cnt = {"ps": 0}

        def psb():
            v = PS[cnt["ps"] % 6]
            cnt["ps"] += 1
            return v

        YSRC = [YAT, YBT, YCT]
        blks = blocks_of(512, 0, L) + ([] if last else blocks_of(512, L, T))
        tcount = 0
        for bi, (p0, bw) in enumerate(blks):
            r = 0 if p0 < L else 1
            nt = bw // 128
            tsl = slice(p0, p0 + bw)
            ub = bi % 2
            for b in range(3):
                S.dma("sp", yin[b][ub][:, :, 0:bw], YSRC[b][:, tsl].rearrange("(c p) t -> p c t", p=128), writes=[yin[b][ub]])
                S.dma("sp", gin[b][:, :, 0:bw], GT[b * D:(b + 1) * D, tsl].rearrange("(c p) t -> p c t", p=128), writes=[gin[b]])
            for dc in range(8):
                pbs = []
                for b in range(3):
                    p = psb()
                    for kc in range(4):
                        mm(p[:, 0:bw], wp[b][:, kc, dc * 128:(dc + 1) * 128], yin[b][ub][:, kc, 0:bw], kc == 0, kc == 3, [wp[b], yin[b][ub]], [p])
                    pbs.append(p)
                for b in range(3):
                    tt("dve", zz[b][:, 0:bw], pbs[b][:, 0:bw], gin[b][:, dc, 0:bw], ALU.mult, [pbs[b], gin[b]], [zz[b]])
                tt("pool", zz[0][:, 0:bw], zz[0][:, 0:bw], zz[1][:, 0:bw], ALU.add, [zz[0], zz[1]], [zz[0]])
                tt("pool", zT[:, dc, 0:bw], zz[0][:, 0:bw], zz[2][:, 0:bw], ALU.add, [zz[0], zz[2]], [(zT, dc)])
            ZT = [(zT, dc) for dc in range(8)]
            h_T = h2T[ub]
            for j in range(nt):
                tix = p0 // 128 + j
                u = tcount % 2
                tcount += 1
                rows = slice(tix * 128, (tix + 1) * 128)
                S.dma("sp", xt[u][:], X[rows, :], reads=[("X", tix)], writes=[xt[u]])
                for half in range(2):
                    p = psb()
                    hs = slice(half * 512, (half + 1) * 512)
                    for kc in range(8):
                        mm(p[:], zT[:, kc, j * 128:(j + 1) * 128], wo[:, kc, hs], kc == 0, kc == 7, ZT + [wo], [p])
                    tt("dve", tmp[u][:, hs], p[:], G1[r][:, hs], ALU.mult, [p, G1[r]], [(tmp[u], half)])
                tt("pool", xn[u][:], xt[u][:], tmp[u][:], ALU.add, [xt[u], (tmp[u], 0), (tmp[u], 1)], [xn[u]])
                S.dma("sp", X[rows, :], xn[u][:], reads=[xn[u]], writes=[("X", tix)])
                act(junk[:], xn[u][:], AF.Square, [xn[u]], [junk, (ss[u], 0)], accum_out=ss[u][:, 0:1])
                rms_scale(ss[u][:, 0:1], ss[u][:, 1:2], float(D), [(ss[u], 0)], [(ss[u], 1)])
                stt("dve", hn[u][:], xn[u][:], ss[u][:, 1:2], A2[r][:], ALU.mult, ALU.mult, [xn[u], (ss[u], 1), A2[r]], [hn[u]])
                tt("pool", hb[u][:], hn[u][:], B2[r][:], ALU.add, [hn[u], B2[r]], [hb[u]])
                pb = PS[6]
                pbb = pb[:].bitcast(BF16)
                for kc in range(8):
                    tr(pbb[:, kc * 128:(kc + 1) * 128], hb[u][:, kc * 128:(kc + 1) * 128], identb[:], [hb[u], identb], [pb])
                cp("act", h_T[:, :, j * 128:(j + 1) * 128], pbb[:].rearrange("p (k t) -> p k t", k=8), [pb], [(h_T, j)])
                R = RT[u]
                pr = PS[7]
                for kc in range(8):
                    mm(pr[:, 0:20], h_T[:, kc, j * 128:(j + 1) * 128], wr[:, kc, :], kc == 0, kc == 7, [(h_T, j), (wr, 0), (wr, 1)], [pr])
                lg, sc = R["lg"], R["s"]
                cp("dve", lg[:], pr[:, 0:20], [pr], [lg])
                S.op("dve", (lambda o, i: (lambda e: e.reduce_max(out=o, in_=i, axis=AX.X)))(sc[:, 0:1], lg[:, 0:4]), [lg], [(sc, 0)])
                ts("dve", R["oh"][:], lg[:, 0:4], sc[:, 0:1], None, ALU.is_equal, None, [lg, (sc, 0)], [R["oh"]])
                ts("dve", sc[:, 1:2], sc[:, 0:1], -1.0, None, ALU.mult, None, [(sc, 0)], [(sc, 1)])
                act(R["eg"][:], lg[:, 0:4], AF.Exp, [lg, (sc, 1)], [R["eg"], (sc, 2)], bias=sc[:, 1:2], scale=1.0, accum_out=sc[:, 2:3])
                recip(sc[:, 3:4], sc[:, 2:3], [(sc, 2)], [(sc, 3)])
                tt("dve", R["prod"][:], lg[:, 4:20].rearrange("p (g e) -> p g e", g=4), R["oh"][:].unsqueeze(2).to_broadcast([128, 4, 4]), ALU.mult,
                   [lg, R["oh"]], [R["prod"]])
                S.op("dve", (lambda o, i: (lambda e: e.reduce_sum(out=o, in_=i, axis=AX.X)))(R["ein"][:], R["prod"][:].rearrange("p g e -> p e g")),
                     [R["prod"]], [R["ein"]])
                S.op("dve", (lambda o, i: (lambda e: e.reduce_max(out=o, in_=i, axis=AX.X)))(sc[:, 4:5], R["ein"][:]), [R["ein"]], [(sc, 4)])
                ts("dve", R["mk1"][:], R["ein"][:], sc[:, 4:5], None, ALU.is_equal, None, [R["ein"], (sc, 4)], [R["mk1"]])
                stt("dve", R["e2"][:], R["mk1"][:], -1e30, R["ein"][:], ALU.mult, ALU.add, [R["mk1"], R["ein"]], [R["e2"]])
                S.op("dve", (lambda o, i: (lambda e: e.reduce_max(out=o, in_=i, axis=AX.X)))(sc[:, 5:6], R["e2"][:]), [R["e2"]], [(sc, 5)])
                ts("dve", R["mk2"][:], R["e2"][:], sc[:, 5:6], None, ALU.is_equal, None, [R["e2"], (sc, 5)], [R["mk2"]])
                tt("dve", sc[:, 6:7], sc[:, 4:5], sc[:, 5:6], ALU.subtract, [(sc, 4), (sc, 5)], [(sc, 6)])
                act(sc[:, 7:8], sc[:, 6:7], AF.Sigmoid, [(sc, 6)], [(sc, 7)])
                act(sc[:, 8:9], sc[:, 6:7], AF.Sigmoid, [(sc, 6)], [(sc, 8)], scale=-1.0)
                tt("dve", sc[:, 7:9], sc[:, 7:9], sc[:, 3:4].to_broadcast([128, 2]), ALU.mult, [(sc, 7), (sc, 8), (sc, 3)], [(sc, 7), (sc, 8)])
                ts("dve", R["gi"][:], R["mk1"][:], sc[:, 7:8], None, ALU.mult, None, [R["mk1"], (sc, 7)], [R["gi"]])
                stt("dve", R["gi"][:], R["mk2"][:], sc[:, 8:9], R["gi"][:], ALU.mult, ALU.add, [R["mk2"], (sc, 8), R["gi"]], [R["gi"]])
                tt("dve", R["gt"][:], R["oh"][:].unsqueeze(2).to_broadcast([128, 4, 4]), R["gi"][:].unsqueeze(1).to_broadcast([128, 4, 4]), ALU.mult,
                   [R["oh"], R["gi"]], [R["gt"]])
                S.dma("sp", GATES[rows, :], R["gt"][:].rearrange("p g e -> p (g e)"), reads=[R["gt"]], writes=[("GATES", tix)])
            S.dma("sp", H2T[:, tsl].rearrange("(k p) t -> p k t", p=128), h_T[:, :, 0:bw], reads=[(h_T, j) for j in range(nt)], writes=[("H2T", bi)])
        A.pop()

    def phase_F(l, last):
        A.push()
        G2 = {}
        for r in ((0,) if last else (0, 1)):
            G2[r] = A.tile(f"G2_{r}", [128, D], F32)
            load_mod_bcast(G2[r], l, r, 5)
        if last:
            gfin = A.tile("gfin", [128, D], F32)
            S.dma("sp", gfin[:], bcast_row(g_final, D), writes=[gfin])
        BW = 1024
        h2 = A.tile("h2F", [128, 8, BW], BF16)
        gts = A.tile("gtsF", [128, BW // 128, 16], F32)
        acc = A.tile("accF", [128, BW // 128, D], F32)
        wA = [A.tile(f"w1F{u}", [128, 8, 512], BF16) for u in range(3)]
        wB = [A.tile(f"w3F{u}", [128, 8, 512], BF16) for u in range(3)]
        wC = [A.tile(f"w2F{u}", [128, 4, D], BF16) for u in range(3)]
        hid = [A.tile(f"hid{u}", [128, 4, BW], BF16) for u in range(2)]
        sa = [A.tile(f"sa{u}", [128, 512], F32) for u in range(2)]
        xt = [A.tile(f"xtF{u}", [128, D], F32) for u in range(2)]
        tmp = [A.tile(f"tmpF{u}", [128, D], F32) for u in range(2)]
        ss = [A.tile(f"ssF{u}", [128, 2], F32) for u in range(2)]
        cnt = {"ps": 0, "sa": 0, "w": 0}

        def psb():
            v = PS[cnt["ps"] % 6]
            cnt["ps"] += 1
            return v

        def load_w(e):
            k = cnt["w"] % 3
            cnt["w"] += 1
            S.dma("sp", wA[k][:], W1B[l, e].rearrange("(k p) n -> p k n", p=128), reads=[("W1B", l, e)], writes=[wA[k]])
            S.dma("sp", wB[k][:], W3B[l, e].rearrange("(k p) n -> p k n", p=128), reads=[("W3B", l, e)], writes=[wB[k]])
            S.dma("sp", wC[k][:], W2B[l, e].rearrange("(k p) n -> p k n", p=128), reads=[("W2B", l, e)], writes=[wC[k]])
            return k

        blks = blocks_of(BW, 0, L) + ([] if last else blocks_of(BW, L, T))
        for bi, (p0, bw) in enumerate(blks):
            r = 0 if p0 < L else 1
            nt = bw // 128
            tsl = slice(p0, p0 + bw)
            hw = min(512, bw)
            S.dma("sp", h2[:, :, 0:bw], H2T[:, tsl].rearrange("(k p) t -> p k t", p=128), writes=[h2])
            S.dma("sp", gts[:, 0:nt, :], GATES[tsl, :].rearrange("(n p) e -> p n e", p=128), writes=[gts])
            wk = {0: load_w(0), 1: load_w(1)}
            for e in range(16):
                if e + 2 < 16:
                    wk[e + 2] = load_w(e + 2)
                k = wk[e]
                hd = hid[e % 2]
                for half in range(bw // hw):
                    ts_ = slice(half * hw, (half + 1) * hw)
                    for cc in range(4):
                        pa, pb = psb(), psb()
                        for kc in range(8):
                            mm(pa[:, 0:hw], wA[k][:, kc, cc * 128:(cc + 1) * 128], h2[:, kc, ts_], kc == 0, kc == 7, [wA[k], h2], [pa])
                        for kc in range(8):
                            mm(pb[:, 0:hw], wB[k][:, kc, cc * 128:(cc + 1) * 128], h2[:, kc, ts_], kc == 0, kc == 7, [wB[k], h2], [pb])
                        s_ = sa[cnt["sa"] % 2]
                        cnt["sa"] += 1
                        act(s_[:, 0:hw], pa[:, 0:hw], AF.Silu, [pa], [s_])
                        tt("dve", hd[:, cc, ts_], s_[:, 0:hw], pb[:, 0:hw], ALU.mult, [s_, pb], [(hd, cc, half)])
                HD = [(hd, cc, half) for cc in range(4) for half in range(bw // hw)]
                for j in range(nt):
                    for half2 in range(2):
                        hs = slice(half2 * 512, (half2 + 1) * 512)
                        p = psb()
                        for cc in range(4):
                            mm(p[:], hd[:, cc, j * 128:(j + 1) * 128], wC[k][:, cc, hs], cc == 0, cc == 3, HD + [wC[k]], [p])
                        if e == 0:
                            ts("dve", acc[:, j, hs], p[:], gts[:, j, e:e + 1], None, ALU.mult, None, [p, gts], [(acc, j, half2)])
                        else:
                            stt("dve", acc[:, j, hs], p[:], gts[:, j, e:e + 1], acc[:, j, hs], ALU.mult, ALU.add, [p, gts, (acc, j, half2)], [(acc, j, half2)])
            for j in range(nt):
                tix = p0 // 128 + j
                u = j % 2
                rows = slice(tix * 128, (tix + 1) * 128)
                S.dma("sp", xt[u][:], X[rows, :], reads=[("X", tix)], writes=[xt[u]])
                tt("pool", tmp[u][:], acc[:, j, :], G2[r][:], ALU.mult, [(acc, j, 0), (acc, j, 1), G2[r]], [tmp[u]])
                tt("pool", xt[u][:], xt[u][:], tmp[u][:], ALU.add, [xt[u], tmp[u]], [xt[u]])
                if last:
                    act(tmp[u][:], xt[u][:], AF.Square, [xt[u]], [tmp[u], (ss[u], 0)], accum_out=ss[u][:, 0:1])
                    rms_scale(ss[u][:, 0:1], ss[u][:, 1:2], float(D), [(ss[u], 0)], [(ss[u], 1)])
                    stt("dve", tmp[u][:], xt[u][:], ss[u][:, 1:2], gfin[:], ALU.mult, ALU.mult, [xt[u], (ss[u], 1), gfin], [tmp[u]])
                    S.dma("sp", out_ap[rows, :], tmp[u][:], reads=[tmp[u]], writes=[("OUT", tix)])
                else:
                    S.dma("sp", X[rows, :], xt[u][:], reads=[xt[u]], writes=[("X", tix)])
        A.pop()

    A.push()
    phase_M(0)
    A.pop()
    S.barrier()
    for l in range(depth):
        last = l == depth - 1
        for fn in (lambda: phase_A(l), lambda: phase_B(l), lambda: phase_C(l, last), lambda: phase_D(l, last), lambda: phase_E(l, last)):
            fn()
            S.barrier()
        A.push()
        if not last:
            phase_M(l + 1)
        phase_F(l, last)
        A.pop()
        S.barrier()
    S.wait_all("sp", [("OUT", t) for t in range(NTL)])
    S.emit()
    st.close()
    return nc, S


_CACHE = {}


def run_module(inputs, L, depth, n_cores=8, debug=False, trace=False, stop_after=None):
    B = inputs["x"].shape[0]
    key = (L, depth, debug, stop_after)
    if key not in _CACHE:
        _CACHE[key] = (build_program(L, depth, debug, stop_after)[0], host_consts(L))
    nc, consts = _CACHE[key]
    in_maps = []
    for core in range(n_cores):
        b = core % B
        m = {}
        m["x"] = np.ascontiguousarray(inputs["x"][b], dtype=np.float32)
        m["ctx"] = np.ascontiguousarray(inputs["ctx"][b], dtype=np.float32)
        cv = np.stack([inputs["c"][b], inputs["c_ctx"]], 0).astype(np.float32)
        m["cvT"] = np.ascontiguousarray(cv.reshape(2, 8, 128).transpose(2, 1, 0))
        for nm in WNAMES:
            a = np.asarray(inputs[nm], dtype=np.float32)
            m[nm] = np.ascontiguousarray(a if nm == "g_final" else a[:depth])
        m.update(consts)
        in_maps.append(m)
    res = run_bass_kernel_spmd(nc, in_maps, core_ids=list(range(n_cores)), trace=trace)
    out = np.stack([res.results[b]["out"] for b in range(B)], 0).astype(np.float32)
    return out, res


def kernel(**inputs):
    out, _ = run_module(inputs, 4096, 4)
    return out
```

```python
import math
from contextlib import ExitStack

import numpy as np
import concourse.bass as bass
import concourse.mybir as mybir
from concourse.bass_utils import run_bass_kernel_spmd

F32 = mybir.dt.float32
BF16 = mybir.dt.bfloat16
AF = mybir.ActivationFunctionType
ALU = mybir.AluOpType
AX = mybir.AxisListType


class Sched:
    EPOCH = 30000
    NDSEM = 24
    SYNC_LAT = 0.9
    DMA_LAT = 2.2

    def __init__(self, nc, same_engine_sync=True, reorder=True):
        self.nc = nc
        self.engs = {"pe": nc.tensor, "dve": nc.vector, "act": nc.scalar, "pool": nc.gpsimd, "sp": nc.sync}
        self.same = same_engine_sync
        self.reorder = reorder
        self.ops = []
        self.segments = []
        self.last_w = {}
        self.readers = {}
        self.n_ins = 0

    @staticmethod
    def _k(r):
        if isinstance(r, (str, int)):
            return r
        if isinstance(r, tuple):
            return tuple(Sched._k(x) for x in r)
        return r.name

    def _record(self, e, fn, kind, reads, writes, dur, lat):
        reads = [self._k(r) for r in reads]
        writes = [self._k(r) for r in writes]
        deps = set()
        for r in reads:
            w = self.last_w.get(r)
            if w is not None:
                deps.add(w)
        for w_ in writes:
            w = self.last_w.get(w_)
            if w is not None:
                deps.add(w)
            rd = self.readers.get(w_)
            if rd:
                deps.update(rd)
        idx = len(self.ops)
        self.ops.append(dict(e=e, fn=fn, kind=kind, deps=deps, dur=dur, lat=lat))
        for r in reads:
            self.readers.setdefault(r, []).append(idx)
        for w_ in writes:
            self.last_w[w_] = idx
            self.readers[w_] = []
        self.n_ins += 1
        return idx

    def op(self, e, fn, reads=(), writes=(), dur=0.3):
        return self._record(e, fn, "c", reads, writes, dur, dur)

    def dma(self, q, out, in_, reads=(), writes=(), nbytes=0, **kw):
        if not nbytes:
            nbytes = 1
            for d in out.shape:
                nbytes *= int(d)
            nbytes *= 2 if out.dtype == BF16 else 4
        lat = self.DMA_LAT + nbytes / 150e3
        return self._record(q, lambda eng: eng.dma_start(out=out, in_=in_, **kw), "d", reads, writes, 0.06, lat)

    def collective(self, kind, op, out, in_, groups, reads=(), writes=()):
        return self._record("pool", lambda eng: eng.collective_compute(kind, op, replica_groups=groups, ins=[in_], outs=[out]),
                            "cc", reads, writes, 0.1, 80.0)

    def wait_all(self, e, resources):
        return self._record(e, None, "w", list(resources), [], 0.01, 0.01)

    def barrier(self):
        if self.ops:
            self.segments.append(self.ops)
        self.ops = []
        self.last_w.clear()
        self.readers.clear()

    def _schedule(self, ops):
        import heapq
        n = len(ops)
        order = {e: [] for e in self.engs}
        if not self.reorder:
            for i, o in enumerate(ops):
                order[o["e"]].append(i)
            return order
        children = [[] for _ in range(n)]
        indeg = [0] * n
        for i, o in enumerate(ops):
            indeg[i] = len(o["deps"])
            for d in o["deps"]:
                children[d].append(i)
        finish = [0.0] * n
        ready_t = [0.0] * n
        ht = {e: [] for e in self.engs}
        ha = {e: [] for e in self.engs}
        tfree = {e: 0.0 for e in self.engs}
        for i, o in enumerate(ops):
            if indeg[i] == 0:
                heapq.heappush(ht[o["e"]], (0.0, i))
        done = 0
        while done < n:
            best = None
            for e in self.engs:
                h, a = ht[e], ha[e]
                tf = tfree[e]
                while h and h[0][0] <= tf:
                    heapq.heappush(a, heapq.heappop(h)[1])
                if a:
                    cand = (tf, a[0], e, True)
                elif h:
                    cand = (h[0][0], h[0][1], e, False)
                else:
                    continue
                if best is None or cand[:2] < best[:2]:
                    best = cand
            start, i, e, from_a = best
            if from_a:
                heapq.heappop(ha[e])
            else:
                heapq.heappop(ht[e])
            o = ops[i]
            tfree[e] = start + o["dur"]
            finish[i] = start + o["lat"]
            order[e].append(i)
            done += 1
            for c in children[i]:
                oc = ops[c]
                t = finish[i] + (self.SYNC_LAT if oc["e"] != e else 0.05)
                if t > ready_t[c]:
                    ready_t[c] = t
                indeg[c] -= 1
                if indeg[c] == 0:
                    heapq.heappush(ht[oc["e"]], (ready_t[c], c))
        return order

    def emit(self):
        nc = self.nc
        self.barrier()
        count = {e: 0 for e in self.engs}
        known = {e: {} for e in self.engs}
        dma_cnt = {}
        dma_i = {}
        semkeys = set()
        prog = {e: [] for e in self.engs}
        for ops in self.segments:
            order = self._schedule(ops)
            ev = [None] * len(ops)
            extra = [None] * len(ops)
            for e in self.engs:
                for i in order[e]:
                    o = ops[i]
                    if o["kind"] == "c":
                        idx = count[e]
                        count[e] += 1
                        key = ("c", e, idx // self.EPOCH)
                        ev[i] = (key, idx % self.EPOCH + 1)
                        semkeys.add(key)
                    elif o["kind"] == "cc":
                        key = ("d", "cc", 0)
                        if dma_cnt.get(key, 0) > 0:
                            extra[i] = (key, 16 * dma_cnt[key])
                        dma_cnt[key] = dma_cnt.get(key, 0) + 1
                        ev[i] = (key, 16 * dma_cnt[key])
                        semkeys.add(key)
                    elif o["kind"] == "d":
                        nsem = self.NDSEM if e == "sp" else 8
                        j = dma_i.get(e, 0) % nsem
                        dma_i[e] = dma_i.get(e, 0) + 1
                        key = ("d", e, j)
                        if dma_cnt.get(key, 0) > 0:
                            extra[i] = (key, 16 * dma_cnt[key])
                        dma_cnt[key] = dma_cnt.get(key, 0) + 1
                        ev[i] = (key, 16 * dma_cnt[key])
                        semkeys.add(key)
            for e in self.engs:
                kn = known[e]
                for i in order[e]:
                    o = ops[i]
                    best = {}
                    for d in o["deps"]:
                        od = ops[d]
                        if ev[d] is None:
                            continue
                        if od["e"] == e and od["kind"] == "c" and o["kind"] == "c" and (e == "pe" or not self.same):
                            continue
                        k, v = ev[d]
                        if best.get(k, 0) < v:
                            best[k] = v
                    if extra[i] is not None:
                        k, v = extra[i]
                        if best.get(k, 0) < v:
                            best[k] = v
                    waits = []
                    for k, v in best.items():
                        if kn.get(k, 0) >= v:
                            continue
                        kn[k] = v
                        waits.append((k, v))
                    if o["kind"] == "w":
                        if waits:
                            prog[e].append((waits, None, None, 0))
                    else:
                        prog[e].append((waits, o["fn"], ev[i][0], 1 if o["kind"] == "c" else 16))
            evs = []
            for e in self.engs:
                if count[e] > 0:
                    evs.append((("c", e, (count[e] - 1) // self.EPOCH), (count[e] - 1) % self.EPOCH + 1))
            for key, c in dma_cnt.items():
                evs.append((key, 16 * c))
            for e in self.engs:
                kn = known[e]
                waits = []
                for k, v in evs:
                    if k[0] == "c" and k[1] == e:
                        continue
                    if kn.get(k, 0) >= v:
                        continue
                    kn[k] = v
                    waits.append((k, v))
                if waits:
                    prog[e].append((waits, None, None, 0))
        self.prog = prog
        with ExitStack() as st:
            sems = {}
            for k in sorted(semkeys, key=str):
                sems[k] = st.enter_context(nc.semaphore("s_" + "_".join(str(x) for x in k)))
            block = st.enter_context(nc.Block())

            def mk(ename):
                def body(eng):
                    for waits, fn, key, inc in prog[ename]:
                        for k, v in waits:
                            eng.wait_ge(sems[k], v)
                        if fn is not None:
                            fn(eng).then_inc(sems[key], inc)
                return body

            block.sync(mk("sp"))
            block.tensor(mk("pe"))
            block.vector(mk("dve"))
            block.scalar(mk("act"))
            block.gpsimd(mk("pool"))


U8 = mybir.dt.uint8
_DTSZ = {F32: 4, BF16: 2}


class Tile:
    def __init__(self, name, ap):
        self.name = name
        self.ap = ap

    def __getitem__(self, k):
        return self.ap[k]


class Arena:
    def __init__(self, nc, st, nbytes=206 * 1024):
        self.t = st.enter_context(nc.sbuf_tensor("arena", [128, nbytes], U8))
        self.nbytes = nbytes
        self.off = 0
        self.marks = []
        self.n = 0

    def push(self):
        self.marks.append(self.off)

    def pop(self):
        self.off = self.marks.pop()

    def tile(self, name, shape, dt):
        free = int(np.prod(shape[1:]))
        nb = (free * _DTSZ[dt] + 31) // 32 * 32
        assert self.off + nb <= self.nbytes, f"SBUF arena overflow at {name}: {self.off}+{nb}"
        v = self.t[0:shape[0], self.off:self.off + free * _DTSZ[dt]].bitcast(dt)
        self.off += nb
        if len(shape) == 3:
            v = v.rearrange("p (a b) -> p a b", a=shape[1])
        elif len(shape) == 4:
            v = v.rearrange("p (a b c) -> p a b c", a=shape[1], b=shape[2])
        self.n += 1
        return Tile(f"{name}#{self.n}", v)


D = 1024
CTX = 256
EPS = 1e-6
IN_COLS = 5888
C_KA, C_VA, C_KC, C_VC, C_QA, C_RA, C_UB, C_QC, C_G = 0, 256, 768, 896, 1024, 1280, 1792, 2304, 2816


def host_consts(L):
    import ml_dtypes
    bf = ml_dtypes.bfloat16
    T = L + CTX
    c = {}
    c["identb"] = np.eye(128, dtype=np.float32).astype(bf)
    c["identf"] = np.eye(128, dtype=np.float32)
    s = np.arange(128)[:, None]
    t = np.arange(128)[None, :]
    same = (s // 64) == (t // 64)
    g = -1.0 / 16.0
    c["tri"] = np.stack([np.where(same & (s <= t), g, 0.0), np.where(same & (s >= t), g, 0.0),
                         np.where(same & (s > t), g, 0.0), np.where(same & (s < t), g, 0.0)], 1).astype(np.float32)
    c["gmask"] = np.stack([np.where(same & (s <= t), 1.0, 0.0), np.where(same & (s >= t), 1.0, 0.0)], 1).astype(bf)
    c["wmask"] = np.stack([np.where(s >= t, 1.0, 0.0), np.where(s <= t, 1.0, 0.0)], 1).astype(bf)
    d = np.arange(128) % 64
    partner = np.where((d % 32) < 16, np.arange(128) + 16, np.arange(128) - 16)
    perm = np.zeros((128, 128), np.float32)
    perm[partner, np.arange(128)] = 1.0
    c["perm"] = perm.astype(bf)
    inv_freq = 10000.0 ** (-np.arange(0, 32, 2, dtype=np.float32) / 32.0)
    pos = np.arange(L)
    row = (pos // 64).astype(np.float32)
    col = (pos % 64).astype(np.float32)
    ang = np.where((d % 64 < 32)[:, None], row[None, :] * inv_freq[d % 16][:, None], col[None, :] * inv_freq[d % 16][:, None]).astype(np.float32)
    cos = np.ones((128, T), np.float32)
    sin = np.zeros((128, T), np.float32)
    cos[:, :L] = np.cos(ang)
    sgn = np.where((d % 32) < 16, -1.0, 1.0)[:, None]
    sin[:, :L] = np.sin(ang) * sgn
    c["rope"] = np.stack([cos, sin], 1).astype(np.float32)
    j = np.arange(128)
    ph = 2.0 * np.pi * np.outer(j, j) / 128.0
    for nm, n in (("cs_lat", L), ("cs_ctx", CTX)):
        sc = 1.0 / math.sqrt(n * 128.0)
        c[nm] = np.concatenate([np.cos(ph) * sc, -np.sin(ph) * sc], 1).astype(bf)
    for nm, n in (("dft_lat", L), ("dft_ctx", CTX)):
        k = np.arange(n, dtype=np.int64)
        m = (np.outer(k, k) % n).astype(np.float64) * (2.0 * np.pi / n)
        c[nm] = np.stack([np.cos(m), np.sin(m)], 0).astype(bf)
    sel = np.zeros((2, 2, 128), np.float32)
    sel[0, 0, :] = 1.0
    sel[1, 1, :] = 1.0
    c["sel2"] = sel
    return c


WNAMES = ["w_ada", "b_ada", "g_mix", "g_ffn", "w_in", "w_decay_down", "w_decay_up", "b_decay", "g_gla", "sink",
          "w_pa", "w_pb", "w_pc", "w_out", "w_router_group", "w_router_expert", "w1", "w3", "w2", "g_final"]


def build_program(L, depth, debug=False, stop_after=None):
    assert L % 512 == 0
    T = L + CTX
    NTL = L // 128
    NT = T // 128
    nc = bass.Bass("TRN2", target_bir_lowering=False)
    S = Sched(nc)

    def din(name, shape, dt=F32):
        return nc.dram_tensor(name, list(shape), dt, kind="ExternalInput").ap()

    def dscr(name, shape, dt):
        return nc.dram_tensor(name, list(shape), dt, kind="ExternalOutput" if debug else "Internal").ap()

    x_in = din("x", [L, D])
    ctx_in = din("ctx", [CTX, D])
    cvT = din("cvT", [128, 8, 2])
    w_ada = din("w_ada", [depth, D, 6 * D])
    b_ada = din("b_ada", [depth, 6 * D])
    g_mix = din("g_mix", [depth, D])
    g_ffn = din("g_ffn", [depth, D])
    w_in = din("w_in", [depth, D, IN_COLS])
    w_dd = din("w_decay_down", [depth, 2, D, 16])
    w_du = din("w_decay_up", [depth, 2, 16, 256])
    b_dec = din("b_decay", [depth, 2, 256])
    g_gla = din("g_gla", [depth, 128])
    sink = din("sink", [depth, 8])
    w_pa = din("w_pa", [depth, 512, D])
    w_pb = din("w_pb", [depth, 512, D])
    w_pc = din("w_pc", [depth, 512, D])
    w_out = din("w_out", [depth, D, D])
    w_rg = din("w_router_group", [depth, D, 4])
    w_re = din("w_router_expert", [depth, D, 16])
    w1 = din("w1", [depth, 16, D, 512])
    w3 = din("w3", [depth, 16, D, 512])
    w2 = din("w2", [depth, 16, 512, D])
    g_final = din("g_final", [D])
    c_identb = din("identb", [128, 128], BF16)
    c_identf = din("identf", [128, 128])
    c_tri = din("tri", [128, 4, 128])
    c_gmask = din("gmask", [128, 2, 128], BF16)
    c_wmask = din("wmask", [128, 2, 128], BF16)
    c_perm = din("perm", [128, 128], BF16)
    c_rope = din("rope", [128, 2, T])
    c_cs_lat = din("cs_lat", [128, 256], BF16)
    c_cs_ctx = din("cs_ctx", [128, 256], BF16)
    c_dft_lat = din("dft_lat", [2, L, L], BF16)
    c_dft_ctx = din("dft_ctx", [2, CTX, CTX], BF16)
    c_sel2 = din("sel2", [2, 2, 128])
    out_ap = nc.dram_tensor("out", [L, D], F32, kind="ExternalOutput").ap()

    X = dscr("X", [T, D], F32)
    MODS = dscr("MODS", [depth, 2, 6 * D], F32)
    QTa = dscr("QTa", [256, T], BF16)
    KTa = dscr("KTa", [256, T], BF16)
    Ka = dscr("Ka", [T, 256], BF16)
    Va = dscr("Va", [T, 512], BF16)
    Ra = dscr("Ra", [T, 512], BF16)
    LA = dscr("LA", [T, 512], F32)
    KTc = dscr("KTc", [128, T], BF16)
    Vc = dscr("Vc", [T, 128], BF16)
    QTc = dscr("QTc", [512, T], BF16)
    UT = dscr("UT", [512, T], BF16)
    GT = dscr("GT", [3072, T], BF16)
    OF = dscr("OF", [T, 512], F32)
    OB = dscr("OB", [T, 512], F32)
    YAT = dscr("YAT", [512, T], BF16)
    YBT = dscr("YBT", [512, T], BF16)
    YCT = dscr("YCT", [512, T], BF16)
    H2T = dscr("H2T", [D, T], BF16)
    GATES = dscr("GATES", [T, 16], F32)
    W1B = nc.dram_tensor("W1B", [depth, 16, D, 512], BF16, kind="Internal").ap()
    W3B = nc.dram_tensor("W3B", [depth, 16, D, 512], BF16, kind="Internal").ap()
    W2B = nc.dram_tensor("W2B", [depth, 16, 512, D], BF16, kind="Internal").ap()

    st = ExitStack()
    A = Arena(nc, st)
    PS = [st.enter_context(nc.psum_tensor(f"ps{i}", [128, 512], F32)) for i in range(8)]

    def _fs(ap):
        n = 1
        for d in ap.shape[1:]:
            n *= int(d)
        return n

    _RATE = {"dve": 0.96e3, "act": 1.2e3, "pool": 0.6e3}

    def _ed(eng, ap):
        return 0.12 + _fs(ap) / _RATE[eng]

    def mm(out, lhsT, rhs, start, stop, rd, wr):
        f32 = lhsT.dtype == F32
        S.op("pe", lambda e: e.matmul(out, lhsT=lhsT, rhs=rhs, start=start, stop=stop), rd, wr,
             dur=0.03 + _fs(out) * (4.0 if f32 else 1.0) / 2.4e3)

    def tr(out, in_, ident, rd, wr):
        S.op("pe", lambda e: e.transpose(out=out, in_=in_, identity=ident), rd, wr, dur=0.03 + _fs(out) / 2.4e3)

    def act(out, in_, func, rd, wr, **kw):
        S.op("act", lambda e: e.activation(out=out, in_=in_, func=func, **kw), rd, wr, dur=_ed("act", out) + 0.1)

    def tt(eng, out, in0, in1, op, rd, wr):
        S.op(eng, lambda e: e.tensor_tensor(out=out, in0=in0, in1=in1, op=op), rd, wr, dur=_ed(eng, out))

    def ts(eng, out, in0, s1, s2, op0, op1, rd, wr):
        if s2 is None:
            S.op(eng, lambda e: e.tensor_scalar(out=out, in0=in0, scalar1=s1, scalar2=None, op0=op0), rd, wr, dur=_ed(eng, out))
        else:
            S.op(eng, lambda e: e.tensor_scalar(out=out, in0=in0, scalar1=s1, scalar2=s2, op0=op0, op1=op1), rd, wr, dur=_ed(eng, out))

    def stt(eng, out, in0, scalar, in1, op0, op1, rd, wr):
        S.op(eng, lambda e: e.scalar_tensor_tensor(out=out, in0=in0, scalar=scalar, in1=in1, op0=op0, op1=op1), rd, wr, dur=_ed(eng, out))

    def cp(eng, out, in_, rd, wr):
        if eng == "act":
            S.op("act", lambda e: e.copy(out=out, in_=in_), rd, wr, dur=_ed("act", out))
        else:
            S.op(eng, lambda e: e.tensor_copy(out=out, in_=in_), rd, wr, dur=_ed(eng, out))

    def amul(out, in_, mul, rd, wr):
        S.op("act", lambda e: e.mul(out=out, in_=in_, mul=mul), rd, wr, dur=_ed("act", out))

    def memset(eng, ap, val, wr):
        S.op(eng, lambda e: e.memset(ap, val), (), wr, dur=_ed(eng, ap))

    def recip(out, in_, rd, wr):
        S.op("dve", lambda e: e.reciprocal(out=out, in_=in_), rd, wr, dur=_ed("dve", out))

    def bcast_row(ap_row, n):
        return ap_row.partition_broadcast(128)

    identb = A.tile("identb", [128, 128], BF16)
    identf = A.tile("identf", [128, 128], F32)
    tri = A.tile("tri", [128, 4, 128], F32)
    gmask = A.tile("gmask", [128, 2, 128], BF16)
    wmask = A.tile("wmask", [128, 2, 128], BF16)
    perm = A.tile("perm", [128, 128], BF16)
    cs_lat = A.tile("cs_lat", [128, 256], BF16)
    cs_ctx = A.tile("cs_ctx", [128, 256], BF16)
    sel2 = A.tile("sel2", [2, 2, 128], F32)
    for tl, src in ((identb, c_identb), (identf, c_identf), (tri, c_tri), (gmask, c_gmask), (wmask, c_wmask),
                    (perm, c_perm), (cs_lat, c_cs_lat), (cs_ctx, c_cs_ctx), (sel2, c_sel2)):
        S.dma("sp", tl[:], src, writes=[tl])

    S.dma("sp", X[0:L, :], x_in, writes=[("X", i) for i in range(NTL)])
    S.dma("sp", X[L:T, :], ctx_in, writes=[("X", i) for i in range(NTL, NT)])

    def blocks_of(width, lo, hi):
        r = []
        p = lo
        while p < hi:
            w = min(width, hi - p)
            r.append((p, w))
            p += w
        return r

    def xkeys(p0, w):
        return [("X", i) for i in range(p0 // 128, (p0 + w) // 128)]

    def rms_scale(ss, rstd, n, rd, wr):
        act(rstd, ss, AF.Sqrt, rd, wr, scale=1.0 / n, bias=EPS)
        recip(rstd, rstd, wr, wr)

    def phase_M(l):
        cT = A.tile("cT", [128, 8, 2], F32)
        cTb = A.tile("cTb", [128, 8, 2], BF16)
        bch = [A.tile(f"bch{i}", [2, 512], F32) for i in range(2)]
        mch = [A.tile(f"mch{i}", [2, 512], F32) for i in range(2)]
        wa = [A.tile(f"wa{i}", [128, 8, 512], BF16) for i in range(2)]
        S.dma("sp", cT[:], cvT, writes=[cT])
        act(cTb[:], cT[:], AF.Silu, [cT], [cTb])
        for cb in range(12):
            w = wa[cb % 2]
            cs_ = slice(cb * 512, (cb + 1) * 512)
            S.dma("pool", w[:], w_ada[l, :, cs_].rearrange("(k p) n -> p k n", p=128), writes=[w])
            bc = bch[cb % 2]
            S.dma("sp", bc[0:1, :], b_ada[l:l + 1, cs_], writes=[(bc, 0)])
            S.dma("sp", bc[1:2, :], b_ada[l:l + 1, cs_], writes=[(bc, 1)])
            pb = PS[6 + cb % 2]
            for kc in range(8):
                mm(pb[0:2, :], cTb[:, kc, :], w[:, kc, :], kc == 0, kc == 7, [cTb, w], [pb])
            mc = mch[cb % 2]
            tt("dve", mc[:], pb[0:2, :], bc[:], ALU.add, [pb, (bc, 0), (bc, 1)], [mc])
            S.dma("sp", MODS[l, :, cs_], mc[:], reads=[mc], writes=[("MODS", l, cb)])

    def conv_thunks(l):
        th = []
        for e in range(16):
            for dst, src, nm in ((W1B, w1, "W1B"), (W3B, w3, "W3B"), (W2B, w2, "W2B")):
                th.append((lambda dst, src, nm, e: (lambda rd: S.dma("pool", dst[l, e], src[l, e], reads=rd, writes=[(nm, l, e)])))(dst, src, nm, e))
        return th

    def load_mod_bcast(dst, l, r, j):
        S.dma("sp", dst[:], bcast_row(MODS[l, r, j * D:(j + 1) * D], D), reads=[("MODS", l, 2 * j), ("MODS", l, 2 * j + 1)], writes=[dst])

    def phase_A(l, conv=()):
        conv = list(conv)
        A.push()
        win = A.tile("win", [128, 8, IN_COLS], BF16)
        S.dma("pool", win[:, 0:4, :], w_in[l, 0:512, :].rearrange("(k p) n -> p k n", p=128), writes=[(win, 0)])
        S.dma("pool", win[:, 4:8, :], w_in[l, 512:1024, :].rearrange("(k p) n -> p k n", p=128), writes=[(win, 1)])
        WIN = [(win, 0), (win, 1)]
        wd = A.tile("wd", [128, 8, 32], BF16)
        for dr in range(2):
            S.dma("pool", wd[:, :, dr * 16:(dr + 1) * 16], w_dd[l, dr].rearrange("(k p) n -> p k n", p=128), writes=[(wd, dr)])
        WD = [(wd, 0), (wd, 1)]
        wu = A.tile("wu", [33, 512], BF16)
        memset("dve", wu[:], 0.0, [wu])
        wu_f = A.tile("wu_f", [33, 512], F32)
        memset("dve", wu_f[:], 0.0, [wu_f])
        S.dma("sp", wu_f[0:16, 0:256], w_du[l, 0], reads=[wu_f], writes=[wu_f])
        S.dma("sp", wu_f[16:32, 256:512], w_du[l, 1], reads=[wu_f], writes=[wu_f])
        S.dma("sp", wu_f[32:33, :], b_dec[l:l + 1].rearrange("o a b -> o (a b)"), reads=[wu_f], writes=[wu_f])
        cp("dve", wu[:], wu_f[:], [wu_f], [wu])
        gm = A.tile("gm", [128, D], F32)
        S.dma("sp", gm[:], bcast_row(g_mix[l], D), writes=[gm])
        A1, B1 = [], []
        for r in range(2):
            a1 = A.tile(f"A1_{r}", [128, D], F32)
            b1 = A.tile(f"B1_{r}", [128, D], F32)
            load_mod_bcast(a1, l, r, 1)
            load_mod_bcast(b1, l, r, 0)
            stt("dve", a1[:], a1[:], 1.0, gm[:], ALU.add, ALU.mult, [a1, gm], [a1])
            A1.append(a1)
            B1.append(b1)
        NB = 2
        xt = [A.tile(f"xt{i}", [128, D], F32) for i in range(NB)]
        junk = A.tile("junkA", [128, D], F32)
        ss = [A.tile(f"ss{i}", [128, 2], F32) for i in range(NB)]
        hn = [A.tile(f"hn{i}", [128, D], F32) for i in range(NB)]
        hb = [A.tile(f"hb{i}", [128, D], BF16) for i in range(NB)]
        hT = [A.tile(f"hT{i}", [128, 8, 512], BF16) for i in range(2)]
        t1T = [A.tile(f"t1T{i}", [33, 512], BF16) for i in range(2)]
        for i in range(2):
            memset("dve", t1T[i][:], 1.0, [t1T[i]])
        NO = 4
        ob = [A.tile(f"ob{i}", [128, 512], BF16) for i in range(NO)]
        of = [A.tile(f"of{i}", [128, 512], F32) for i in range(NO)]
        rp = [A.tile(f"rp{i}", [128, 2, 512], F32) for i in range(2)]
        ra = [A.tile(f"ra{i}", [128, 512], F32) for i in range(2)]
        rb = [A.tile(f"rb{i}", [128, 512], F32) for i in range(2)]
        cnt = {"ob": 0, "of": 0, "ps": 0, "r": 0}

        def nxt(k, n):
            v = cnt[k] % n
            cnt[k] += 1
            return v

        def psb():
            return PS[nxt("ps", 6)]

        blks = blocks_of(512, 0, L) + blocks_of(512, L, T)

        def prep(bi):
            p0, bw = blks[bi]
            r = 0 if p0 < L else 1
            nt = bw // 128
            h_T = hT[bi % 2]
            for j in range(nt):
                tix = p0 // 128 + j
                u = (bi * 4 + j) % NB
                S.dma("sp", xt[u][:], X[tix * 128:(tix + 1) * 128, :], reads=[("X", tix)], writes=[xt[u]])
                act(junk[:], xt[u][:], AF.Square, [xt[u]], [junk, (ss[u], 0)], accum_out=ss[u][:, 0:1])
                rms_scale(ss[u][:, 0:1], ss[u][:, 1:2], float(D), [(ss[u], 0)], [(ss[u], 1)])
                stt("dve", hn[u][:], xt[u][:], ss[u][:, 1:2], A1[r][:], ALU.mult, ALU.mult, [xt[u], (ss[u], 1), A1[r]], [hn[u]])
                tt("pool", hb[u][:], hn[u][:], B1[r][:], ALU.add, [hn[u], B1[r]], [hb[u]])
                pb = psb()
                pbb = pb[:].bitcast(BF16)
                for kc in range(8):
                    tr(pbb[:, kc * 128:(kc + 1) * 128], hb[u][:, kc * 128:(kc + 1) * 128], identb[:], [hb[u], identb], [pb])
                cp("act", h_T[:, :, j * 128:(j + 1) * 128], pbb[:].rearrange("p (k t) -> p k t", k=8), [pb], [(h_T, j)])

        def part1(bi):
            p0, bw = blks[bi]
            r = 0 if p0 < L else 1
            nt = bw // 128
            h_T = hT[bi % 2]
            HT = [(h_T, j) for j in range(nt)]
            tsl = slice(p0, p0 + bw)
            rpt = rp[bi % 2]
            S.dma("sp", rpt[:, :, 0:bw], c_rope[:, :, p0:p0 + bw], writes=[rpt])

            def fm(col0, dst_cb):
                pb = psb()
                for kc in range(8):
                    mm(pb[:, 0:bw], win[:, kc, col0:col0 + 128], h_T[:, kc, 0:bw], kc == 0, kc == 7, WIN + HT, [pb])
                dst_cb(pb)

            def fm_plain(dram, row0, scale, key):
                def cb(pb):
                    o = ob[nxt("ob", NO)]
                    if scale == 1.0:
                        cp("act", o[:, 0:bw], pb[:, 0:bw], [pb], [o])
                    else:
                        amul(o[:, 0:bw], pb[:, 0:bw], scale, [pb], [o])
                    S.dma("sp", dram[row0:row0 + 128, tsl], o[:, 0:bw], reads=[o], writes=[(key, row0, bi)])
                return cb

            def fm_rope(dram, row0, scale, key):
                def cb(pb):
                    o = ob[nxt("ob", NO)]
                    amul(o[:, 0:bw], pb[:, 0:bw], scale, [pb], [o])
                    p2 = psb()
                    mm(p2[:, 0:bw], perm[:], o[:, 0:bw], True, True, [perm, o], [p2])
                    k = nxt("r", 2)
                    tt("dve", ra[k][:, 0:bw], o[:, 0:bw], rpt[:, 0, 0:bw], ALU.mult, [o, rpt], [ra[k]])
                    tt("dve", rb[k][:, 0:bw], p2[:, 0:bw], rpt[:, 1, 0:bw], ALU.mult, [p2, rpt], [rb[k]])
                    o2 = ob[nxt("ob", NO)]
                    tt("pool", o2[:, 0:bw], ra[k][:, 0:bw], rb[k][:, 0:bw], ALU.add, [ra[k], rb[k]], [o2])
                    S.dma("sp", dram[row0:row0 + 128, tsl], o2[:, 0:bw], reads=[o2], writes=[(key, row0, bi)])
                return cb

            def fm_sig(row0):
                def cb(pb):
                    o = ob[nxt("ob", NO)]
                    act(o[:, 0:bw], pb[:, 0:bw], AF.Sigmoid, [pb], [o])
                    S.dma("sp", GT[row0:row0 + 128, tsl], o[:, 0:bw], reads=[o], writes=[("GT", row0, bi)])
                return cb

            for c2 in range(2):
                fm(C_KA + c2 * 128, fm_plain(KTa, c2 * 128, 1.0, "KTa"))
            for c2 in range(2):
                fm(C_QA + c2 * 128, fm_plain(QTa, c2 * 128, 0.125, "QTa"))
            fm(C_KC, fm_rope(KTc, 0, 1.0, "KTc"))
            for c4 in range(4):
                fm(C_QC + c4 * 128, fm_rope(QTc, c4 * 128, 0.125, "QTc"))
            for c4 in range(4):
                fm(C_UB + c4 * 128, fm_plain(UT, c4 * 128, 1.0, "UT"))
            for c24 in range(24):
                fm(C_G + c24 * 128, fm_sig(c24 * 128))

        def part2(bi):
            p0, bw = blks[bi]
            r = 0 if p0 < L else 1
            nt = bw // 128
            h_T = hT[bi % 2]
            HT = [(h_T, j) for j in range(nt)]
            tsl = slice(p0, p0 + bw)
            pb = psb()
            for kc in range(8):
                mm(pb[0:32, 0:bw], wd[:, kc, :], h_T[:, kc, 0:bw], kc == 0, kc == 7, WD + HT, [pb])
            t1 = t1T[bi % 2]
            cp("act", t1[0:32, 0:bw], pb[0:32, 0:bw], [pb], [t1])
            for _ in range(6):
                if conv:
                    conv.pop(0)([t1])
            for j in range(nt):
                tix = p0 // 128 + j
                rows = slice(tix * 128, (tix + 1) * 128)
                lhs = lambda kc: h_T[:, kc, j * 128:(j + 1) * 128]
                pb = psb()
                for kc in range(8):
                    mm(pb[:], lhs(kc), win[:, kc, 0:512], kc == 0, kc == 7, WIN + HT, [pb])
                o = ob[nxt("ob", NO)]
                cp("act", o[:], pb[:], [pb], [o])
                S.dma("sp", Ka[rows, :], o[:, 0:256], reads=[o], writes=[("Ka", tix)])
                S.dma("sp", Va[rows, 0:256], o[:, 256:512], reads=[o], writes=[("Va0", tix)])
                pb = psb()
                for kc in range(8):
                    mm(pb[:], lhs(kc), win[:, kc, 512:1024], kc == 0, kc == 7, WIN + HT, [pb])
                o = ob[nxt("ob", NO)]
                cp("dve", o[:], pb[:], [pb], [o])
                S.dma("sp", Va[rows, 256:512], o[:, 0:256], reads=[o], writes=[("Va1", tix)])
                S.dma("sp", Vc[rows, :], o[:, 384:512], reads=[o], writes=[("Vc", tix)])
                pb = psb()
                for kc in range(8):
                    mm(pb[:], lhs(kc), win[:, kc, C_RA:C_RA + 512], kc == 0, kc == 7, WIN + HT, [pb])
                o = ob[nxt("ob", NO)]
                cp("act", o[:], pb[:], [pb], [o])
                S.dma("sp", Ra[rows, :], o[:], reads=[o], writes=[("Ra", tix)])
                pb = psb()
                mm(pb[:], t1[0:33, j * 128:(j + 1) * 128], wu[:], True, True, [t1, wu], [pb])
                f = of[nxt("of", NO)]
                act(f[:], pb[:], AF.Exp, [pb], [f], scale=-1.0)
                act(f[:], f[:], AF.Ln, [f], [f], bias=1.0, scale=1.0)
                S.dma("sp", LA[rows, :], f[:], reads=[f], writes=[("LA", tix)])

        prep(0)
        for bi in range(len(blks)):
            part1(bi)
            if bi + 1 < len(blks):
                prep(bi + 1)
            part2(bi)
        while conv:
            conv.pop(0)([])
        A.pop()

    def phase_B(l):
        A.push()
        S32 = A.tile("S32", [128, 4, 128], F32)
        Sbf = A.tile("Sbf", [128, 4, 128], BF16)
        tmpS = A.tile("tmpS", [128, 4, 128], F32)
        memset("dve", S32[:], 0.0, [S32])
        memset("dve", Sbf[:], 0.0, [Sbf])
        Sbf2 = [Sbf, A.tile("Sbf_b", [128, 4, 128], BF16)]
        qT2 = [A.tile(f"qT2{u}", [128, 4, 128], BF16) for u in range(2)]
        kT2 = [A.tile(f"kT2{u}", [128, 4, 128], BF16) for u in range(2)]
        kx = [[A.tile(f"kx{d}{u}", [128, 256], BF16) for u in range(2)] for d in range(2)]
        vx = [[A.tile(f"vx{d}{u}", [128, 512], BF16) for u in range(2)] for d in range(2)]
        lax = [[A.tile(f"lax{d}{u}", [128, 256], F32) for u in range(2)] for d in range(2)]
        E1 = [A.tile(f"E1{u}", [128, 4, 128], F32) for u in range(2)]
        E2 = [A.tile(f"E2{u}", [128, 4, 128], F32) for u in range(2)]
        ED = [A.tile(f"ED{u}", [128, 512], F32) for u in range(2)]
        qe = [A.tile(f"qe{u}", [128, 4, 128], BF16) for u in range(2)]
        ke = [A.tile(f"ke{u}", [128, 4, 128], BF16) for u in range(2)]
        kd = [[A.tile(f"kd{d}{u}", [128, 256], BF16) for u in range(2)] for d in range(2)]
        scT = [[A.tile(f"scT{d}{u}", [128, 4, 128], BF16) for u in range(2)] for d in range(2)]
        EL = [A.tile(f"EL{u}", [128, 2, 4], F32) for u in range(2)]
        osb = [[A.tile(f"osb{d}{u}", [128, 512], F32) for u in range(2)] for d in range(2)]
        Pc, Pd, Pst = PS[0], PS[1], PS[6]
        Ps = [PS[2], PS[2]]
        Po = [PS[3], PS[4]]
        Pi = [PS[5], PS[7]]
        order = [[NTL, NTL + 1] + list(range(NTL)), [NTL + 1, NTL] + list(range(NTL - 1, -1, -1))]
        ODR = [OF, OB]

        def v3(ap):
            return ap.rearrange("p (h t) -> p h t", h=4)

        def prelude(i):
            u = i % 2
            tx = [order[0][i], order[1][i]]
            for d in range(2):
                cols = slice(tx[d] * 128, (tx[d] + 1) * 128)
                rows = cols
                S.dma("sp", qT2[u][d * 64:(d + 1) * 64, :, :], QTa[:, cols].rearrange("(h k) t -> k h t", h=4), writes=[(qT2[u], d)])
                S.dma("sp", kT2[u][d * 64:(d + 1) * 64, :, :], KTa[:, cols].rearrange("(h k) t -> k h t", h=4), writes=[(kT2[u], d)])
                S.dma("sp", kx[d][u][:], Ka[rows, :], writes=[kx[d][u]])
                S.dma("sp", vx[d][u][:], Va[rows, :], writes=[vx[d][u]])
                S.dma("sp", lax[d][u][:], LA[rows, d * 256:(d + 1) * 256], writes=[lax[d][u]])
            for d in range(2):
                for h in range(4):
                    mm(Pc[d * 64:(d + 1) * 64, h * 128:(h + 1) * 128], lax[d][u][:, h * 64:(h + 1) * 64], tri[:, d, :], True, True,
                       [lax[d][u], tri], [Pc])
            for d in range(2):
                mm(Pd[:, d * 256:(d + 1) * 256], tri[:, 2 + d, :], lax[d][u][:], True, True, [lax[d][u], tri], [Pd])
            act(E1[u][:], v3(Pc[:]), AF.Exp, [Pc], [E1[u]])
            act(E2[u][:], v3(Pc[:]), AF.Exp, [Pc], [E2[u]], scale=-1.0)
            act(ED[u][:], Pd[:], AF.Exp, [Pd], [ED[u]])
            tt("dve", qe[u][:], qT2[u][:], E1[u][:], ALU.mult, [(qT2[u], 0), (qT2[u], 1), E1[u]], [qe[u]])
            tt("pool", ke[u][:], kT2[u][:], E2[u][:], ALU.mult, [(kT2[u], 0), (kT2[u], 1), E2[u]], [ke[u]])
            tt("dve", kd[0][u][:], kx[0][u][:], ED[u][:, 0:256], ALU.mult, [kx[0][u], ED[u]], [kd[0][u]])
            tt("pool", kd[1][u][:], kx[1][u][:], ED[u][:, 256:512], ALU.mult, [kx[1][u], ED[u]], [kd[1][u]])
            for ci in range(2):
                cf, cb = ci, 1 - ci
                cp("pool", EL[u][0:64, ci, :], E1[u][0:64, :, cf * 64 + 63], [E1[u]], [(EL[u], ci, 0)])
                cp("pool", EL[u][64:128, ci, :], E1[u][64:128, :, cb * 64], [E1[u]], [(EL[u], ci, 1)])
            for d in range(2):
                for h in range(4):
                    for cx in range(2):
                        cs_ = slice(cx * 64, (cx + 1) * 64)
                        mm(Ps[d][cs_, h * 128 + cx * 64:h * 128 + (cx + 1) * 64], ke[u][d * 64:(d + 1) * 64, h, cs_], qe[u][d * 64:(d + 1) * 64, h, cs_],
                           True, True, [ke[u], qe[u]], [Ps[d]])
                tt("dve", scT[d][u][:], v3(Ps[d][:]), gmask[:, d, :].unsqueeze(1).to_broadcast([128, 4, 128]), ALU.mult,
                   [Ps[d], gmask], [scT[d][u]])
        def chain(i):
            u = i % 2
            tx = [order[0][i], order[1][i]]
            for ci in range(2):
                sb_in = Sbf2[(2 * i + ci) % 2]
                sb_out = Sbf2[(2 * i + ci + 1) % 2]
                for d in range(2):
                    cx = ci if d == 0 else 1 - ci
                    cs_ = slice(cx * 64, (cx + 1) * 64)
                    for h in range(4):
                        mm(Pst[d * 64:(d + 1) * 64, h * 128:(h + 1) * 128], kd[d][u][cs_, h * 64:(h + 1) * 64], vx[d][u][cs_, h * 128:(h + 1) * 128],
                           True, True, [kd[d][u], vx[d][u]], [Pst])
                tt("dve", tmpS[:], S32[:], EL[u][:, ci, :].unsqueeze(2).to_broadcast([128, 4, 128]), ALU.mult,
                   [S32, (EL[u], ci, 0), (EL[u], ci, 1)], [tmpS])
                tt("dve", S32[:], tmpS[:], v3(Pst[:]), ALU.add, [tmpS, Pst], [S32])
                cp("act", sb_out[:], S32[:], [S32], [sb_out])
                for d in range(2):
                    cx = ci if d == 0 else 1 - ci
                    cs_ = slice(cx * 64, (cx + 1) * 64)
                    ds_ = slice(d * 64, (d + 1) * 64)
                    for h in range(4):
                        hs = slice(h * 128, (h + 1) * 128)
                        mm(Po[d][cs_, hs], scT[d][u][cs_, h, cs_], vx[d][u][cs_, hs], True, True, [scT[d][u], vx[d][u]], [Po[d]])
                    for h in range(4):
                        hs = slice(h * 128, (h + 1) * 128)
                        mm(Pi[d][cs_, hs], qe[u][ds_, h, cs_], sb_in[ds_, h, :], True, True, [qe[u], sb_in], [Pi[d]])
            for d in range(2):
                cp("act", osb[d][u][:], Po[d][:], [Po[d]], [osb[d][u]])
                tt("dve", osb[d][u][:], Pi[d][:], osb[d][u][:], ALU.add, [Pi[d], osb[d][u]], [osb[d][u]])
                S.dma("sp", ODR[d][tx[d] * 128:(tx[d] + 1) * 128, :], osb[d][u][:], reads=[osb[d][u]], writes=[("O", d, tx[d])])
        prelude(0)
        for i in range(NT):
            if i + 1 < NT:
                prelude(i + 1)
            chain(i)
        GG = A.tile("GG", [128, 128], F32)
        S.dma("sp", GG[:], bcast_row(g_gla[l], 128), writes=[GG])
        o1 = [A.tile(f"o1{u}", [128, 512], F32) for u in range(2)]
        o2 = [A.tile(f"o2{u}", [128, 512], F32) for u in range(2)]
        rr = [A.tile(f"rr{u}", [128, 512], BF16) for u in range(2)]
        osum = [A.tile(f"osum{u}", [128, 4, 128], F32) for u in range(2)]
        junk = A.tile("junkB", [128, 128], F32)
        ssg = [A.tile(f"ssg{u}", [128, 8], F32) for u in range(2)]
        sr = [A.tile(f"sr{u}", [128, 512], F32) for u in range(2)]
        t1 = [A.tile(f"t1{u}", [128, 4, 128], F32) for u in range(2)]
        yb_ = [A.tile(f"yb{u}", [128, 512], BF16) for u in range(2)]
        yT = [A.tile(f"yT{u}", [128, 4, 128], BF16) for u in range(2)]
        for t in range(NT):
            u = t % 2
            rows = slice(t * 128, (t + 1) * 128)
            S.dma("sp", o1[u][:], OF[rows, :], reads=[("O", 0, t)], writes=[o1[u]])
            S.dma("sp", o2[u][:], OB[rows, :], reads=[("O", 1, t)], writes=[o2[u]])
            S.dma("sp", rr[u][:], Ra[rows, :], writes=[rr[u]])
            tt("pool", osum[u][:], v3(o1[u][:]), v3(o2[u][:]), ALU.add, [o1[u], o2[u]], [osum[u]])
            for h in range(4):
                act(junk[:], osum[u][:, h, :], AF.Square, [osum[u]], [junk, (ssg[u], h)], accum_out=ssg[u][:, h:h + 1])
            rms_scale(ssg[u][:, 0:4], ssg[u][:, 4:8], 128.0, [(ssg[u], h) for h in range(4)], [(ssg[u], 9)])
            tt("dve", t1[u][:], osum[u][:], ssg[u][:, 4:8].unsqueeze(2).to_broadcast([128, 4, 128]), ALU.mult, [osum[u], (ssg[u], 9)], [t1[u]])
            act(sr[u][:], rr[u][:], AF.Silu, [rr[u]], [sr[u]])
            tt("pool", t1[u][:], t1[u][:], GG[:].unsqueeze(1).to_broadcast([128, 4, 128]), ALU.mult, [t1[u], GG], [t1[u]])
            tt("dve", yb_[u][:], t1[u][:].rearrange("p h t -> p (h t)"), sr[u][:], ALU.mult, [t1[u], sr[u]], [yb_[u]])
            pb = PS[t % 2]
            pbb = pb[:].bitcast(BF16)
            for c4 in range(4):
                tr(pbb[:, c4 * 128:(c4 + 1) * 128], yb_[u][:, c4 * 128:(c4 + 1) * 128], identb[:], [yb_[u], identb], [pb])
            cp("act", yT[u][:], pbb[:, 0:512].rearrange("p (c t) -> p c t", c=4), [pb], [yT[u]])
            S.dma("sp", YAT[:, rows].rearrange("(c p) t -> p c t", p=128), yT[u][:], reads=[yT[u]], writes=[("YAT", t)])
        A.pop()

    def phase_C(l, last):
        A.push()
        for which, n, t0, cs, dft in (("lat", L, 0, cs_lat, c_dft_lat), ("ctx", CTX, L, cs_ctx, c_dft_ctx)):
            if which == "ctx" and last:
                continue
            ntl = n // 128
            AB = A.tile("AB", [128, ntl, 4, 256], BF16)
            uT = [A.tile(f"uT{u}", [128, 4, 128], BF16) for u in range(2)]
            for t in range(ntl):
                u = t % 2
                S.dma("sp", uT[u][:], UT[:, t0 + t * 128:t0 + (t + 1) * 128].rearrange("(g c) t -> c g t", g=4), writes=[uT[u]])
                P0, P1 = PS[(t % 2) * 2], PS[(t % 2) * 2 + 1]
                for g in range(4):
                    pg = P0 if g < 2 else P1
                    mm(pg[:, (g % 2) * 256:(g % 2 + 1) * 256], uT[u][:, g, :], cs[:], True, True, [uT[u], cs], [pg])
                cp("act", AB[:, t, 0:2, :], P0[:].rearrange("p (g c) -> p g c", g=2), [P0], [(AB, t, 0)])
                cp("dve", AB[:, t, 2:4, :], P1[:].rearrange("p (g c) -> p g c", g=2), [P1], [(AB, t, 1)])
            ABK = [(AB, t, q) for t in range(ntl) for q in range(2)]
            GRP = min(8, ntl)
            pbw = min(512, n)
            cl = [A.tile(f"cl{u}", [128, GRP, pbw], BF16) for u in range(3)]
            sl = [A.tile(f"sl{u}", [128, GRP, pbw], BF16) for u in range(3)]
            ybt = [A.tile(f"ybt{u}", [128, 4, pbw], BF16) for u in range(2)]
            k3 = 0
            for pb_ in range(n // pbw):
                Pg = [PS[4 + g] for g in range(4)]
                for lq in range(ntl // GRP):
                    c_, s_ = cl[k3 % 3], sl[k3 % 3]
                    k3 += 1
                    rws = slice(lq * GRP * 128, (lq + 1) * GRP * 128)
                    cls = slice(pb_ * pbw, (pb_ + 1) * pbw)
                    S.dma("sp", c_[:], dft[0, rws, cls].rearrange("(k p) n -> p k n", p=128), writes=[c_])
                    S.dma("sp", s_[:], dft[1, rws, cls].rearrange("(k p) n -> p k n", p=128), writes=[s_])
                    for g in range(4):
                        for k in range(GRP):
                            lt = lq * GRP + k
                            mm(Pg[g][:, 0:pbw], AB[:, lt, g, 0:128], c_[:, k, :], lt == 0, False, ABK + [c_], [Pg[g]])
                            mm(Pg[g][:, 0:pbw], AB[:, lt, g, 128:256], s_[:, k, :], False, lt == ntl - 1, ABK + [s_], [Pg[g]])
                yo = ybt[pb_ % 2]
                for g in range(4):
                    cp("act" if g % 2 == 0 else "dve", yo[:, g, :], Pg[g][:, 0:pbw], [Pg[g]], [(yo, g)])
                S.dma("sp", YBT[:, t0 + pb_ * pbw:t0 + (pb_ + 1) * pbw].rearrange("(g j) t -> j g t", g=4), yo[:],
                      reads=[(yo, g) for g in range(4)], writes=[("YBT", which, pb_)])
        A.pop()

    def phase_D(l, last):
        A.push()
        kTa = A.tile("kTall", [64, 2, T], BF16)
        Vg = A.tile("Vaug", [128, NT, 2, 65], BF16)
        ES = A.tile("ES", [128, 8], F32)
        S.dma("sp", kTa[:], KTc.rearrange("(v k) t -> k v t", v=2), writes=[kTa])
        memset("pool", Vg[:], 1.0, [Vg])
        for t in range(NT):
            S.dma("sp", Vg[:, t, :, 0:64], Vc[t * 128:(t + 1) * 128, :].rearrange("p (v k) -> p v k", v=2), reads=[Vg], writes=[Vg])
        S.dma("sp", ES[:], bcast_row(sink[l], 8), writes=[ES])
        act(ES[:], ES[:], AF.Exp, [ES], [ES])
        qt = [A.tile(f"qt{u}", [64, 8 * 128], BF16) for u in range(2)]
        pT = [A.tile(f"pT{u}", [128, 512], BF16) for u in range(12)]
        den = [A.tile(f"den{u}", [128, 8], F32) for u in range(2)]
        yc = [A.tile(f"yc{u}", [128, 8, 64], BF16) for u in range(2)]
        ycT = [A.tile(f"ycT{u}", [128, 4, 128], BF16) for u in range(2)]
        nblk = NTL if last else NT
        kpc = {"kp": 0}

        def body(n):
            u = n % 2
            cols = slice(n * 128, (n + 1) * 128)
            S.dma("sp", qt[u][:].rearrange("k (h t) -> k h t", h=8), QTc[:, cols].rearrange("(h k) t -> k h t", h=8), writes=[qt[u]])
            if n < NTL:
                chunks = ([(n - 1, 0)] if n > 0 else []) + [(n, None)] + ([(n + 1, 1)] if n < NTL - 1 else []) + [(NTL, None), (NTL + 1, None)]
            else:
                chunks = [(NTL, None), (NTL + 1, None)]
            Pv = [PS[3 + u * 2], PS[4 + u * 2]]
            for v in range(2):
                pts = []
                for ci, (kt, mk) in enumerate(chunks):
                    kp = kpc["kp"]
                    pk = PS[kp % 3]
                    p_ = pT[kp % 12]
                    kpc["kp"] += 1
                    mm(pk[:], kTa[:, v, kt * 128:(kt + 1) * 128], qt[u][:, v * 512:(v + 1) * 512], True, True, [kTa, qt[u]], [pk])
                    act(p_[:], pk[:], AF.Exp, [pk], [p_])
                    if mk is not None:
                        tt("pool", p_[:].rearrange("p (h t) -> p h t", h=4), p_[:].rearrange("p (h t) -> p h t", h=4),
                           wmask[:, mk, :].unsqueeze(1).to_broadcast([128, 4, 128]), ALU.mult, [p_, wmask], [p_])
                    pts.append(p_)
                for h in range(4):
                    for ci, (kt, mk) in enumerate(chunks):
                        mm(Pv[v][:, h * 65:(h + 1) * 65], pts[ci][:, h * 128:(h + 1) * 128], Vg[:, kt, v, :], ci == 0, ci == len(chunks) - 1,
                           [pts[ci], Vg], [Pv[v]])

        def tail(n):
            u = n % 2
            cols = slice(n * 128, (n + 1) * 128)
            Pv = [PS[3 + u * 2], PS[4 + u * 2]]
            for v in range(2):
                acc = Pv[v][:, 0:260].rearrange("p (h c) -> p h c", h=4)
                tt("dve", den[u][:, v * 4:(v + 1) * 4], acc[:, :, 64], ES[:, v * 4:(v + 1) * 4], ALU.add, [Pv[v], ES], [(den[u], v)])
                recip(den[u][:, v * 4:(v + 1) * 4], den[u][:, v * 4:(v + 1) * 4], [(den[u], v)], [(den[u], v)])
                tt("dve", yc[u][:, v * 4:(v + 1) * 4, :], acc[:, :, 0:64], den[u][:, v * 4:(v + 1) * 4].unsqueeze(2).to_broadcast([128, 4, 64]),
                   ALU.mult, [Pv[v], (den[u], v)], [(yc[u], v)])
            pb = PS[7]
            pbb = pb[:].bitcast(BF16)
            ycf = yc[u][:].rearrange("p h k -> p (h k)")
            for c4 in range(4):
                tr(pbb[:, c4 * 128:(c4 + 1) * 128], ycf[:, c4 * 128:(c4 + 1) * 128], identb[:], [(yc[u], 0), (yc[u], 1), identb], [pb])
            cp("dve", ycT[u][:], pbb[:, 0:512].rearrange("p (c t) -> p c t", c=4), [pb], [ycT[u]])
            S.dma("sp", YCT[:, cols].rearrange("(c p) t -> p c t", p=128), ycT[u][:], reads=[ycT[u]], writes=[("YCT", n)])

        for n in range(nblk):
            body(n)
            if n > 0:
                tail(n - 1)
        tail(nblk - 1)
        A.pop()

    def phase_E(l, last):
        A.push()
        wp = []
        for nm, src in (("wpa", w_pa), ("wpb", w_pb), ("wpc", w_pc)):
            w = A.tile(nm, [128, 4, D], BF16)
            S.dma("pool", w[:], src[l].rearrange("(k p) n -> p k n", p=128), writes=[w])
            wp.append(w)
        wo = A.tile("wo", [128, 8, D], BF16)
        S.dma("pool", wo[:], w_out[l].rearrange("(k p) n -> p k n", p=128), writes=[wo])
        wr = A.tile("wr", [128, 8, 20], BF16)
        S.dma("pool", wr[:, :, 0:4], w_rg[l].rearrange("(k p) n -> p k n", p=128), writes=[(wr, 0)])
        S.dma("pool", wr[:, :, 4:20], w_re[l].rearrange("(k p) n -> p k n", p=128), writes=[(wr, 1)])
        gf = A.tile("gf", [128, D], F32)
        S.dma("sp", gf[:], bcast_row(g_ffn[l], D), writes=[gf])
        G1, A2, B2 = {}, {}, {}
        for r in ((0,) if last else (0, 1)):
            G1[r] = A.tile(f"G1_{r}", [128, D], F32)
            A2[r] = A.tile(f"A2_{r}", [128, D], F32)
            B2[r] = A.tile(f"B2_{r}", [128, D], F32)
            load_mod_bcast(G1[r], l, r, 2)
            load_mod_bcast(B2[r], l, r, 3)
            load_mod_bcast(A2[r], l, r, 4)
            stt("dve", A2[r][:], A2[r][:], 1.0, gf[:], ALU.add, ALU.mult, [A2[r], gf], [A2[r]])
        yin = [[A.tile(f"yin{b}{u}", [128, 4, 512], BF16) for u in range(2)] for b in range(3)]
        gin = [A.tile(f"gin{b}", [128, 8, 512], BF16) for b in range(3)]
        zz = [A.tile(f"zz{i}", [128, 512], F32) for i in range(3)]
        zT = A.tile("zT", [128, 8, 512], BF16)
        xt = [A.tile(f"xtE{u}", [128, D], F32) for u in range(2)]
        tmp = [A.tile(f"tmpE{u}", [128, D], F32) for u in range(2)]
        xn = [A.tile(f"xn{u}", [128, D], F32) for u in range(2)]
        junk = A.tile("junkE", [128, D], F32)
        ss = [A.tile(f"ssE{u}", [128, 2], F32) for u in range(2)]
        hn = [A.tile(f"hnE{u}", [128, D], F32) for u in range(2)]
        hb = [A.tile(f"hbE{u}", [128, D], BF16) for u in range(2)]
        h2T = [A.tile(f"h2T{u}", [128, 8, 512], BF16) for u in range(2)]
        RT = [dict(lg=A.tile(f"lg{u}", [128, 20], F32), s=A.tile(f"rs{u}", [128, 16], F32), oh=A.tile(f"oh{u}", [128, 4], F32),
                   eg=A.tile(f"eg{u}", [128, 4], F32), prod=A.tile(f"prod{u}", [128, 4, 4], F32), ein=A.tile(f"ein{u}", [128, 4], F32),
                   mk1=A.tile(f"mk1{u}", [128, 4], F32), e2=A.tile(f"e2{u}", [128, 4], F32), mk2=A.tile(f"mk2{u}", [128, 4], F32),
                   gi=A.tile(f"gi{u}", [128, 4], F32), gt=A.tile(f"gt{u}", [128, 4, 4], F32)) for u in range(2)]
        cnt = {"ps": 0}

        def psb():
            v = PS[cnt["ps"] % 6]
            cnt["ps"] += 1
            return v

        YSRC = [YAT, YBT, YCT]
        blks = blocks_of(512, 0, L) + ([] if last else blocks_of(512, L, T))
        tcount = 0
        for bi, (p0, bw) in enumerate(blks):
            r = 0 if p0 < L else 1
            nt = bw // 128
            tsl = slice(p0, p0 + bw)
            ub = bi % 2
            for b in range(3):
                S.dma("sp", yin[b][ub][:, :, 0:bw], YSRC[b][:, tsl].rearrange("(c p) t -> p c t", p=128), writes=[yin[b][ub]])
                S.dma("sp", gin[b][:, :, 0:bw], GT[b * D:(b + 1) * D, tsl].rearrange("(c p) t -> p c t", p=128), writes=[gin[b]])
            for dc in range(8):
                pbs = []
                for b in range(3):
                    p = psb()
                    for kc in range(4):
                        mm(p[:, 0:bw], wp[b][:, kc, dc * 128:(dc + 1) * 128], yin[b][ub][:, kc, 0:bw], kc == 0, kc == 3, [wp[b], yin[b][ub]], [p])
                    pbs.append(p)
                for b in range(3):
                    tt("dve", zz[b][:, 0:bw], pbs[b][:, 0:bw], gin[b][:, dc, 0:bw], ALU.mult, [pbs[b], gin[b]], [zz[b]])
                tt("pool", zz[0][:, 0:bw], zz[0][:, 0:bw], zz[1][:, 0:bw], ALU.add, [zz[0], zz[1]], [zz[0]])
                tt("pool", zT[:, dc, 0:bw], zz[0][:, 0:bw], zz[2][:, 0:bw], ALU.add, [zz[0], zz[2]], [(zT, dc)])
            ZT = [(zT, dc) for dc in range(8)]
            h_T = h2T[ub]
            for j in range(nt):
                tix = p0 // 128 + j
                u = tcount % 2
                tcount += 1
                rows = slice(tix * 128, (tix + 1) * 128)
                S.dma("sp", xt[u][:], X[rows, :], reads=[("X", tix)], writes=[xt[u]])
                for half in range(2):
                    p = psb()
                    hs = slice(half * 512, (half + 1) * 512)
                    for kc in range(8):
                        mm(p[:], zT[:, kc, j * 128:(j + 1) * 128], wo[:, kc, hs], kc == 0, kc == 7, ZT + [wo], [p])
                    tt("dve", tmp[u][:, hs], p[:], G1[r][:, hs], ALU.mult, [p, G1[r]], [(tmp[u], half)])
                tt("pool", xn[u][:], xt[u][:], tmp[u][:], ALU.add, [xt[u], (tmp[u], 0), (tmp[u], 1)], [xn[u]])
                S.dma("sp", X[rows, :], xn[u][:], reads=[xn[u]], writes=[("X", tix)])
                act(junk[:], xn[u][:], AF.Square, [xn[u]], [junk, (ss[u], 0)], accum_out=ss[u][:, 0:1])
                rms_scale(ss[u][:, 0:1], ss[u][:, 1:2], float(D), [(ss[u], 0)], [(ss[u], 1)])
                stt("dve", hn[u][:], xn[u][:], ss[u][:, 1:2], A2[r][:], ALU.mult, ALU.mult, [xn[u], (ss[u], 1), A2[r]], [hn[u]])
                tt("pool", hb[u][:], hn[u][:], B2[r][:], ALU.add, [hn[u], B2[r]], [hb[u]])
                pb = PS[6]
                pbb = pb[:].bitcast(BF16)
                for kc in range(8):
                    tr(pbb[:, kc * 128:(kc + 1) * 128], hb[u][:, kc * 128:(kc + 1) * 128], identb[:], [hb[u], identb], [pb])
                cp("act", h_T[:, :, j * 128:(j + 1) * 128], pbb[:].rearrange("p (k t) -> p k t", k=8), [pb], [(h_T, j)])
                R = RT[u]
                pr = PS[7]
                for kc in range(8):
                    mm(pr[:, 0:20], h_T[:, kc, j * 128:(j + 1) * 128], wr[:, kc, :], kc == 0, kc == 7, [(h_T, j), (wr, 0), (wr, 1)], [pr])
                lg, sc = R["lg"], R["s"]
                cp("dve", lg[:], pr[:, 0:20], [pr], [lg])
                S.op("dve", (lambda o, i: (lambda e: e.reduce_max(out=o, in_=i, axis=AX.X)))(sc[:, 0:1], lg[:, 0:4]), [lg], [(sc, 0)])
                ts("dve", R["oh"][:], lg[:, 0:4], sc[:, 0:1], None, ALU.is_equal, None, [lg, (sc, 0)], [R["oh"]])
                ts("dve", sc[:, 1:2], sc[:, 0:1], -1.0, None, ALU.mult, None, [(sc, 0)], [(sc, 1)])
                act(R["eg"][:], lg[:, 0:4], AF.Exp, [lg, (sc, 1)], [R["eg"], (sc, 2)], bias=sc[:, 1:2], scale=1.0, accum_out=sc[:, 2:3])
                recip(sc[:, 3:4], sc[:, 2:3], [(sc, 2)], [(sc, 3)])
                tt("dve", R["prod"][:], lg[:, 4:20].rearrange("p (g e) -> p g e", g=4), R["oh"][:].unsqueeze(2).to_broadcast([128, 4, 4]), ALU.mult,
                   [lg, R["oh"]], [R["prod"]])
                S.op("dve", (lambda o, i: (lambda e: e.reduce_sum(out=o, in_=i, axis=AX.X)))(R["ein"][:], R["prod"][:].rearrange("p g e -> p e g")),
                     [R["prod"]], [R["ein"]])
                S.op("dve", (lambda o, i: (lambda e: e.reduce_max(out=o, in_=i, axis=AX.X)))(sc[:, 4:5], R["ein"][:]), [R["ein"]], [(sc, 4)])
                ts("dve", R["mk1"][:], R["ein"][:], sc[:, 4:5], None, ALU.is_equal, None, [R["ein"], (sc, 4)], [R["mk1"]])
                stt("dve", R["e2"][:], R["mk1"][:], -1e30, R["ein"][:], ALU.mult, ALU.add, [R["mk1"], R["ein"]], [R["e2"]])
                S.op("dve", (lambda o, i: (lambda e: e.reduce_max(out=o, in_=i, axis=AX.X)))(sc[:, 5:6], R["e2"][:]), [R["e2"]], [(sc, 5)])
                ts("dve", R["mk2"][:], R["e2"][:], sc[:, 5:6], None, ALU.is_equal, None, [R["e2"], (sc, 5)], [R["mk2"]])
                tt("dve", sc[:, 6:7], sc[:, 4:5], sc[:, 5:6], ALU.subtract, [(sc, 4), (sc, 5)], [(sc, 6)])
                act(sc[:, 7:8], sc[:, 6:7], AF.Sigmoid, [(sc, 6)], [(sc, 7)])
                act(sc[:, 8:9], sc[:, 6:7], AF.Sigmoid, [(sc, 6)], [(sc, 8)], scale=-1.0)
                tt("dve", sc[:, 7:9], sc[:, 7:9], sc[:, 3:4].to_broadcast([128, 2]), ALU.mult, [(sc, 7), (sc, 8), (sc, 3)], [(sc, 7), (sc, 8)])
                ts("dve", R["gi"][:], R["mk1"][:], sc[:, 7:8], None, ALU.mult, None, [R["mk1"], (sc, 7)], [R["gi"]])
                stt("dve", R["gi"][:], R["mk2"][:], sc[:, 8:9], R["gi"][:], ALU.mult, ALU.add, [R["mk2"], (sc, 8), R["gi"]], [R["gi"]])
                tt("dve", R["gt"][:], R["oh"][:].unsqueeze(2).to_broadcast([128, 4, 4]), R["gi"][:].unsqueeze(1).to_broadcast([128, 4, 4]), ALU.mult,
                   [R["oh"], R["gi"]], [R["gt"]])
                S.dma("sp", GATES[rows, :], R["gt"][:].rearrange("p g e -> p (g e)"), reads=[R["gt"]], writes=[("GATES", tix)])
            S.dma("sp", H2T[:, tsl].rearrange("(k p) t -> p k t", p=128), h_T[:, :, 0:bw], reads=[(h_T, j) for j in range(nt)], writes=[("H2T", bi)])
        A.pop()

    def phase_F(l, last, conv=()):
        conv = list(conv)
        A.push()
        G2 = {}
        for r in ((0,) if last else (0, 1)):
            G2[r] = A.tile(f"G2_{r}", [128, D], F32)
            load_mod_bcast(G2[r], l, r, 5)
        if last:
            gfin = A.tile("gfin", [128, D], F32)
            S.dma("sp", gfin[:], bcast_row(g_final, D), writes=[gfin])
        BW = 1024
        h2 = A.tile("h2F", [128, 8, BW], BF16)
        gts = A.tile("gtsF", [128, BW // 128, 16], F32)
        acc = A.tile("accF", [128, BW // 128, D], F32)
        wA = [A.tile(f"w1F{u}", [128, 8, 512], BF16) for u in range(3)]
        wB = [A.tile(f"w3F{u}", [128, 8, 512], BF16) for u in range(3)]
        wC = [A.tile(f"w2F{u}", [128, 4, D], BF16) for u in range(3)]
        hid = [A.tile(f"hid{u}", [128, 4, BW], BF16) for u in range(2)]
        sa = [A.tile(f"sa{u}", [128, 512], F32) for u in range(2)]
        xt = [A.tile(f"xtF{u}", [128, D], F32) for u in range(2)]
        tmp = [A.tile(f"tmpF{u}", [128, D], F32) for u in range(2)]
        ss = [A.tile(f"ssF{u}", [128, 2], F32) for u in range(2)]
        cnt = {"ps": 0, "sa": 0, "w": 0}

        def psb():
            v = PS[cnt["ps"] % 6]
            cnt["ps"] += 1
            return v

        def load_w(e):
            k = cnt["w"] % 3
            cnt["w"] += 1
            S.dma("sp", wA[k][:], W1B[l, e].rearrange("(k p) n -> p k n", p=128), reads=[("W1B", l, e)], writes=[wA[k]])
            S.dma("sp", wB[k][:], W3B[l, e].rearrange("(k p) n -> p k n", p=128), reads=[("W3B", l, e)], writes=[wB[k]])
            S.dma("sp", wC[k][:], W2B[l, e].rearrange("(k p) n -> p k n", p=128), reads=[("W2B", l, e)], writes=[wC[k]])
            return k

        blks = blocks_of(BW, 0, L) + ([] if last else blocks_of(BW, L, T))
        for bi, (p0, bw) in enumerate(blks):
            r = 0 if p0 < L else 1
            nt = bw // 128
            tsl = slice(p0, p0 + bw)
            hw = min(512, bw)
            S.dma("sp", h2[:, :, 0:bw], H2T[:, tsl].rearrange("(k p) t -> p k t", p=128), writes=[h2])
            S.dma("sp", gts[:, 0:nt, :], GATES[tsl, :].rearrange("(n p) e -> p n e", p=128), writes=[gts])
            wk = {0: load_w(0), 1: load_w(1)}
            for e in range(16):
                if e + 2 < 16:
                    wk[e + 2] = load_w(e + 2)
                k = wk[e]
                hd = hid[e % 2]
                for half in range(bw // hw):
                    ts_ = slice(half * hw, (half + 1) * hw)
                    for cc in range(4):
                        pa, pb = psb(), psb()
                        for kc in range(8):
                            mm(pa[:, 0:hw], wA[k][:, kc, cc * 128:(cc + 1) * 128], h2[:, kc, ts_], kc == 0, kc == 7, [wA[k], h2], [pa])
                        for kc in range(8):
                            mm(pb[:, 0:hw], wB[k][:, kc, cc * 128:(cc + 1) * 128], h2[:, kc, ts_], kc == 0, kc == 7, [wB[k], h2], [pb])
                        s_ = sa[cnt["sa"] % 2]
                        cnt["sa"] += 1
                        act(s_[:, 0:hw], pa[:, 0:hw], AF.Silu, [pa], [s_])
                        tt("dve", hd[:, cc, ts_], s_[:, 0:hw], pb[:, 0:hw], ALU.mult, [s_, pb], [(hd, cc, half)])
                HD = [(hd, cc, half) for cc in range(4) for half in range(bw // hw)]
                for j in range(nt):
                    for half2 in range(2):
                        hs = slice(half2 * 512, (half2 + 1) * 512)
                        p = psb()
                        for cc in range(4):
                            mm(p[:], hd[:, cc, j * 128:(j + 1) * 128], wC[k][:, cc, hs], cc == 0, cc == 3, HD + [wC[k]], [p])
                        if e == 0:
                            ts("dve", acc[:, j, hs], p[:], gts[:, j, e:e + 1], None, ALU.mult, None, [p, gts], [(acc, j, half2)])
                        else:
                            stt("dve", acc[:, j, hs], p[:], gts[:, j, e:e + 1], acc[:, j, hs], ALU.mult, ALU.add, [p, gts, (acc, j, half2)], [(acc, j, half2)])
                if conv:
                    conv.pop(0)([(acc, 0, 0)])
            for j in range(nt):
                tix = p0 // 128 + j
                u = j % 2
                rows = slice(tix * 128, (tix + 1) * 128)
                S.dma("sp", xt[u][:], X[rows, :], reads=[("X", tix)], writes=[xt[u]])
                tt("pool", tmp[u][:], acc[:, j, :], G2[r][:], ALU.mult, [(acc, j, 0), (acc, j, 1), G2[r]], [tmp[u]])
                tt("pool", xt[u][:], xt[u][:], tmp[u][:], ALU.add, [xt[u], tmp[u]], [xt[u]])
                if last:
                    act(tmp[u][:], xt[u][:], AF.Square, [xt[u]], [tmp[u], (ss[u], 0)], accum_out=ss[u][:, 0:1])
                    rms_scale(ss[u][:, 0:1], ss[u][:, 1:2], float(D), [(ss[u], 0)], [(ss[u], 1)])
                    stt("dve", tmp[u][:], xt[u][:], ss[u][:, 1:2], gfin[:], ALU.mult, ALU.mult, [xt[u], (ss[u], 1), gfin], [tmp[u]])
                    S.dma("sp", out_ap[rows, :], tmp[u][:], reads=[tmp[u]], writes=[("OUT", tix)])
                else:
                    S.dma("sp", X[rows, :], xt[u][:], reads=[xt[u]], writes=[("X", tix)])
        while conv:
            conv.pop(0)([])
        A.pop()

    A.push()
    phase_M(0)
    A.pop()
    S.barrier()
    for l in range(depth):
        last = l == depth - 1
        for fn in (lambda: phase_A(l, conv_thunks(0) if l == 0 else ()), lambda: phase_B(l), lambda: phase_C(l, last), lambda: phase_D(l, last), lambda: phase_E(l, last)):
            fn()
            S.barrier()
        A.push()
        if not last:
            phase_M(l + 1)
        phase_F(l, last, () if last else conv_thunks(l + 1))
        A.pop()
        S.barrier()
    S.wait_all("sp", [("OUT", t) for t in range(NTL)])
    S.emit()
    st.close()
    return nc, S


_CACHE = {}


def run_module(inputs, L, depth, n_cores=8, debug=False, trace=False, stop_after=None):
    B = inputs["x"].shape[0]
    key = (L, depth, debug, stop_after)
    if key not in _CACHE:
        _CACHE[key] = (build_program(L, depth, debug, stop_after)[0], host_consts(L))
    nc, consts = _CACHE[key]
    in_maps = []
    for core in range(n_cores):
        b = core % B
        m = {}
        m["x"] = np.ascontiguousarray(inputs["x"][b], dtype=np.float32)
        m["ctx"] = np.ascontiguousarray(inputs["ctx"][b], dtype=np.float32)
        cv = np.stack([inputs["c"][b], inputs["c_ctx"]], 0).astype(np.float32)
        m["cvT"] = np.ascontiguousarray(cv.reshape(2, 8, 128).transpose(2, 1, 0))
        for nm in WNAMES:
            a = np.asarray(inputs[nm], dtype=np.float32)
            m[nm] = np.ascontiguousarray(a if nm == "g_final" else a[:depth])
        m.update(consts)
        in_maps.append(m)
    res = run_bass_kernel_spmd(nc, in_maps, core_ids=list(range(n_cores)), trace=trace)
    out = np.stack([res.results[b]["out"] for b in range(B)], 0).astype(np.float32)
    return out, res


def kernel(**inputs):
    out, _ = run_module(inputs, 4096, 4)
    return out
```
